# Optimizing a Trainium2 kernel written in Bass

```python
import jax
import jax.numpy as jnp
from jax import lax
import numpy as np

D_MODEL = 1024
BATCH = 8
SEQ = 4096
DEPTH = 2

GRID_W = 64
CTX_LEN = 256
CHUNK = 128
A_GROUP_DIM = 128
A_GROUPS = D_MODEL // A_GROUP_DIM
D_A = A_GROUPS * A_GROUP_DIM
R_HEADS = 4
R_QK_DIM = D_MODEL // 8
R_V_DIM = D_MODEL // 4
D_RQK = R_HEADS * R_QK_DIM
D_RV = R_HEADS * R_V_DIM
D_IN = 2 * D_A + 2 * D_RQK + 2 * D_RV + 2 * D_MODEL
KV_START = 2 * D_A + D_RQK
KV_END = KV_START + D_RQK + D_RV
ROPE_BASE = 10000.0
MOE_GROUPS = 4
EXPERTS_PER_GROUP = 8
N_EXPERTS = MOE_GROUPS * EXPERTS_PER_GROUP
TOP_K = 2
EXPERT_FF = D_MODEL // 2
MOE_BLOCK = 128
EPS = 1e-6

kernel_name = 'hybrid_sgu_retention_hmoe_dit'


def rmsnorm(x, g):
    xf = x.astype(jnp.float32)
    y = xf * lax.rsqrt(jnp.mean(xf * xf, axis=-1, keepdims=True) + EPS)
    return (y * g.astype(jnp.float32)).astype(x.dtype)


def layernorm(x):
    xf = x.astype(jnp.float32)
    mu = jnp.mean(xf, axis=-1, keepdims=True)
    var = jnp.mean(jnp.square(xf - mu), axis=-1, keepdims=True)
    return ((xf - mu) * lax.rsqrt(var + EPS)).astype(x.dtype)


def adaln(cond, w_ada, b_ada):
    m = jax.nn.silu(cond) @ w_ada + b_ada
    return jnp.split(m[..., None, :], 6, axis=-1)


def modulate(h, shift, scale):
    return h * (1.0 + scale) + shift


def to_heads(t, n_heads):
    b, n, _ = t.shape
    return t.reshape(b, n, n_heads, -1).transpose(0, 2, 1, 3)


def grid_rotary(rows):
    n_freq = R_QK_DIM // 4
    inv = ROPE_BASE ** (-jnp.arange(n_freq, dtype=jnp.float32) / n_freq)
    row = jnp.repeat(jnp.arange(rows, dtype=jnp.float32), GRID_W)
    col = jnp.tile(jnp.arange(GRID_W, dtype=jnp.float32), rows)
    ang = jnp.concatenate([row[:, None] * inv, col[:, None] * inv], axis=-1)
    return jnp.cos(ang), jnp.sin(ang)


def apply_rotary(t, cos, sin):
    t1, t2 = t[..., 0::2], t[..., 1::2]
    r = jnp.stack([t1 * cos - t2 * sin, t1 * sin + t2 * cos], axis=-1)
    return r.reshape(t.shape).astype(t.dtype)


def retention_direction(q, k, v, log_g, s0, inclusive):
    b, h, n, dk = q.shape
    dv = v.shape[-1]
    nc = n // CHUNK
    qc = q.reshape(b, h, nc, CHUNK, dk)
    kc = k.reshape(b, h, nc, CHUNK, dk)
    vc = v.reshape(b, h, nc, CHUNK, dv)
    j = jnp.arange(CHUNK, dtype=jnp.float32)
    diff = j[:, None] - j[None, :]
    keep = (diff >= 0) if inclusive else (diff > 0)
    d_intra = jnp.where(keep[None], jnp.exp(log_g[:, None, None] * jnp.maximum(diff, 0.0)[None]), 0.0)
    q_dec = jnp.exp(log_g[:, None] * (j + 1.0))
    k_dec = jnp.exp(log_g[:, None] * (CHUNK - 1.0 - j))
    c_dec = jnp.exp(log_g * CHUNK)[None, :, None, None]
    scores = jnp.einsum('bhncd,bhnmd->bhncm', qc, kc) * d_intra[None, :, None]
    intra = jnp.einsum('bhncm,bhnme->bhnce', scores, vc)
    kv = jnp.einsum('bhncd,bhnce->nbhde', kc * k_dec[None, :, None, :, None], vc)

    def step(s, kv_i):
        return s * c_dec + kv_i, s

    _, s_prev = lax.scan(step, s0, kv)
    cross = jnp.einsum('bhncd,nbhde->bhnce', qc * q_dec[None, :, None, :, None], s_prev)
    return (intra + cross).reshape(b, h, n, dv)


def retention_bidirectional(q, k, v, log_gf, log_gb, s0_f, s0_b):
    flip = lambda t: jnp.flip(t, axis=2)
    o_f = retention_direction(q, k, v, log_gf, s0_f, True)
    o_b = retention_direction(flip(q), flip(k), flip(v), log_gb, s0_b, False)
    return o_f + flip(o_b)


def context_state(k, v, log_g, reverse):
    n = k.shape[2]
    m = jnp.arange(n, dtype=jnp.float32)
    expo = m if reverse else (n - 1.0) - m
    w = jnp.exp(log_g[:, None] * expo[None, :])
    return jnp.einsum('bhmd,bhme,hm->bhde', k, v, w)


def spatial_gating(u, v, w_s, b_s, g_sgu):
    b, n, _ = u.shape
    nc = n // CHUNK
    vn = (layernorm(v) * g_sgu).reshape(b, nc, CHUNK, A_GROUPS, A_GROUP_DIM)
    s = jnp.einsum('gpq,bnqgc->bnpgc', w_s, vn) + b_s.T[None, None, :, :, None]
    return u * s.reshape(b, n, D_A)


def split_projection(p):
    sizes = (D_A, D_A, D_RQK, D_RQK, D_RV, D_RV, D_MODEL, D_MODEL)
    cuts = [int(s) for s in np.cumsum(sizes)[:-1]]
    return jnp.split(p, cuts, axis=-1)


def token_mix(p, s0_f, s0_b, log_gf, log_gb, rotary, w_s, b_s, g_sgu, w_pa, w_pb, w_out):
    u, va, q, k, vr, gr, ga, gb = split_projection(p)
    y_a = spatial_gating(jax.nn.gelu(u), jax.nn.gelu(va), w_s, b_s, g_sgu)
    qh = to_heads(q, R_HEADS).astype(jnp.float32)
    kh = to_heads(k, R_HEADS).astype(jnp.float32) * (R_QK_DIM ** -0.5)
    vh = to_heads(vr, R_HEADS).astype(jnp.float32)
    if rotary is not None:
        cos, sin = rotary
        qh = apply_rotary(qh, cos, sin)
        kh = apply_rotary(kh, cos, sin)
    o = retention_bidirectional(qh, kh, vh, log_gf, log_gb, s0_f, s0_b)
    o = layernorm(o).transpose(0, 2, 1, 3).reshape(p.shape[0], p.shape[1], D_RV).astype(p.dtype)
    y_b = jax.nn.silu(gr) * o
    merged = jax.nn.sigmoid(ga) * (y_a @ w_pa) + jax.nn.sigmoid(gb) * (y_b @ w_pb)
    return merged @ w_out


def expert_dispatch(h, expert_id, weights, w1, w3, w2):
    t, d = h.shape
    a = t * TOP_K
    e_flat = expert_id.reshape(a)
    tok_flat = jnp.repeat(jnp.arange(t, dtype=jnp.int32), TOP_K)
    w_flat = weights.reshape(a)
    order = jnp.argsort(e_flat)
    e_sorted, tok_sorted, w_sorted = e_flat[order], tok_flat[order], w_flat[order]
    counts = jnp.bincount(e_flat, length=N_EXPERTS)
    padded = (counts + MOE_BLOCK - 1) // MOE_BLOCK * MOE_BLOCK
    start = jnp.cumsum(counts) - counts
    pend = jnp.cumsum(padded)
    dest = (pend - padded)[e_sorted] + (jnp.arange(a) - start[e_sorted])
    n_blocks = (a + N_EXPERTS * (MOE_BLOCK - 1) + MOE_BLOCK - 1) // MOE_BLOCK
    p = n_blocks * MOE_BLOCK
    buf_tok = jnp.full((p,), t, dtype=jnp.int32).at[dest].set(tok_sorted)
    buf_w = jnp.zeros((p,), h.dtype).at[dest].set(w_sorted.astype(h.dtype))
    blk_e = jnp.minimum(jnp.searchsorted(pend, jnp.arange(n_blocks) * MOE_BLOCK, side='right'), N_EXPERTS - 1)
    h_pad = jnp.concatenate([h, jnp.zeros((1, d), h.dtype)], axis=0)
    xb = h_pad[buf_tok].reshape(n_blocks, MOE_BLOCK, d)

    def expert_block(args):
        xblk, e = args
        return (jax.nn.silu(xblk @ w1[e]) * (xblk @ w3[e])) @ w2[e]

    yb = lax.map(expert_block, (xb, blk_e))
    y = yb.reshape(p, d) * buf_w[:, None]
    return jax.ops.segment_sum(y, buf_tok, num_segments=t + 1)[:t]


def hier_moe(h, w_group, b_group, w_erouter, b_erouter, w1, w3, w2):
    t = h.shape[0]
    hf = h.astype(jnp.float32)
    g_logits = hf @ w_group.astype(jnp.float32) + b_group.astype(jnp.float32)
    g_top, g_sel = lax.top_k(g_logits, 1)
    p_group = jnp.exp(g_top - jax.nn.logsumexp(g_logits, axis=-1, keepdims=True))
    e_logits = (hf @ w_erouter.astype(jnp.float32) + b_erouter.astype(jnp.float32)).reshape(t, MOE_GROUPS, EXPERTS_PER_GROUP)
    idx = jnp.broadcast_to(g_sel[:, :, None], (t, 1, EXPERTS_PER_GROUP))
    e_in_group = jnp.take_along_axis(e_logits, idx, axis=1)[:, 0]
    e_top, e_sel = lax.top_k(e_in_group, TOP_K)
    weights = p_group * jax.nn.softmax(e_top, axis=-1)
    expert_id = g_sel * EXPERTS_PER_GROUP + e_sel
    return expert_dispatch(h, expert_id, weights, w1, w3, w2)


def hybrid_layer(x, xc, c, c_ctx, w_ada, b_ada, g_mix, g_ffn, w_in, w_s, b_s, g_sgu, decay_logit,
                 w_pa, w_pb, w_out, w_group, b_group, w_erouter, b_erouter, w1, w3, w2, rotary, update_ctx):
    b, n, d = x.shape
    l_ctx = xc.shape[1]
    sh1, sc1, gt1, sh2, sc2, gt2 = adaln(c, w_ada, b_ada)
    sh1c, sc1c, gt1c, sh2c, sc2c, gt2c = adaln(c_ctx, w_ada, b_ada)
    log_g = jax.nn.log_sigmoid(decay_logit.astype(jnp.float32))
    log_gf, log_gb = log_g[0], log_g[1]
    mix_w = (w_s, b_s, g_sgu, w_pa, w_pb, w_out)
    h = modulate(rmsnorm(x, g_mix), sh1, sc1)
    hc = modulate(rmsnorm(xc, g_mix), sh1c, sc1c)
    if update_ctx:
        pc = hc @ w_in
        kv_c = pc[..., KV_START:KV_END]
    else:
        kv_c = hc @ w_in[:, KV_START:KV_END]
    kh_c = to_heads(kv_c[..., :D_RQK], R_HEADS).astype(jnp.float32) * (R_QK_DIM ** -0.5)
    vh_c = to_heads(kv_c[..., D_RQK:], R_HEADS).astype(jnp.float32)
    s_ctx_f = context_state(kh_c, vh_c, log_gf, False)
    s_ctx_b = context_state(kh_c, vh_c, log_gb, True)
    x = x + gt1 * token_mix(h @ w_in, s_ctx_f, s_ctx_b, log_gf, log_gb, rotary, *mix_w)
    if update_ctx:
        zero_state = jnp.zeros_like(s_ctx_f)
        xc = xc + gt1c * token_mix(pc, zero_state, zero_state, log_gf, log_gb, None, *mix_w)
    h2 = modulate(rmsnorm(x, g_ffn), sh2, sc2).reshape(b * n, d)
    if update_ctx:
        h2c = modulate(rmsnorm(xc, g_ffn), sh2c, sc2c).reshape(b * l_ctx, d)
        y = hier_moe(jnp.concatenate([h2, h2c], axis=0), w_group, b_group, w_erouter, b_erouter, w1, w3, w2)
        x = x + gt2 * y[:b * n].reshape(b, n, d)
        xc = xc + gt2c * y[b * n:].reshape(b, l_ctx, d)
    else:
        y = hier_moe(h2, w_group, b_group, w_erouter, b_erouter, w1, w3, w2)
        x = x + gt2 * y.reshape(b, n, d)
    return x, xc


def setup_inputs(seed: int = 0) -> dict:
    key = jax.random.key(seed)
    ks = jax.random.split(key, 24)
    f32 = jnp.float32
    d = D_MODEL

    def nrm(k, shape, s):
        return jax.random.normal(k, shape, f32) * s

    base_logit = jnp.log(2.0 ** (5.0 + jnp.arange(R_HEADS, dtype=f32)) - 1.0)
    return {
        'x': nrm(ks[0], (BATCH, SEQ, d), 1.0),
        'c': nrm(ks[1], (BATCH, d), 1.0),
        'ctx': nrm(ks[2], (BATCH, CTX_LEN, d), 1.0),
        'c_ctx': nrm(ks[3], (d,), 1.0),
        'w_ada': nrm(ks[4], (DEPTH, d, 6 * d), 0.5 * d ** -0.5),
        'b_ada': nrm(ks[5], (DEPTH, 6 * d), 0.02),
        'g_mix': 1.0 + nrm(ks[6], (DEPTH, d), 0.02),
        'g_ffn': 1.0 + nrm(ks[7], (DEPTH, d), 0.02),
        'w_in': nrm(ks[8], (DEPTH, d, D_IN), d ** -0.5),
        'w_s': nrm(ks[9], (DEPTH, A_GROUPS, CHUNK, CHUNK), CHUNK ** -0.5),
        'b_s': 1.0 + nrm(ks[10], (DEPTH, A_GROUPS, CHUNK), 0.02),
        'g_sgu': 1.0 + nrm(ks[11], (DEPTH, D_A), 0.02),
        'decay_logit': base_logit + nrm(ks[12], (DEPTH, 2, R_HEADS), 0.1),
        'w_pa': nrm(ks[13], (DEPTH, D_A, d), D_A ** -0.5),
        'w_pb': nrm(ks[14], (DEPTH, D_RV, d), D_RV ** -0.5),
        'w_out': nrm(ks[15], (DEPTH, d, d), d ** -0.5),
        'w_group': nrm(ks[16], (DEPTH, d, MOE_GROUPS), d ** -0.5),
        'b_group': nrm(ks[17], (DEPTH, MOE_GROUPS), 0.01),
        'w_erouter': nrm(ks[18], (DEPTH, d, N_EXPERTS), d ** -0.5),
        'b_erouter': nrm(ks[19], (DEPTH, N_EXPERTS), 0.01),
        'w1': nrm(ks[20], (DEPTH, N_EXPERTS, d, EXPERT_FF), d ** -0.5),
        'w3': nrm(ks[21], (DEPTH, N_EXPERTS, d, EXPERT_FF), d ** -0.5),
        'w2': nrm(ks[22], (DEPTH, N_EXPERTS, EXPERT_FF, d), EXPERT_FF ** -0.5),
        'g_final': 1.0 + nrm(ks[23], (d,), 0.02),
    }


def reference(x, c, ctx, c_ctx, w_ada, b_ada, g_mix, g_ffn, w_in, w_s, b_s, g_sgu, decay_logit,
              w_pa, w_pb, w_out, w_group, b_group, w_erouter, b_erouter, w1, w3, w2, g_final):
    rows = x.shape[1] // GRID_W
    rotary = grid_rotary(rows)
    xc = ctx
    for l in range(DEPTH):
        x, xc = hybrid_layer(x, xc, c, c_ctx, w_ada[l], b_ada[l], g_mix[l], g_ffn[l], w_in[l], w_s[l], b_s[l],
                             g_sgu[l], decay_logit[l], w_pa[l], w_pb[l], w_out[l], w_group[l], b_group[l],
                             w_erouter[l], b_erouter[l], w1[l], w3[l], w2[l], rotary, l < DEPTH - 1)
    return rmsnorm(x, g_final)
```

```python
import numpy as np
from contextlib import ExitStack
import concourse.bass as bass
import concourse.mybir as mybir
from concourse.bass_utils import run_bass_kernel_spmd

F32 = mybir.dt.float32
BF16 = mybir.dt.bfloat16
I32 = mybir.dt.int32
AF = mybir.ActivationFunctionType
ALU = mybir.AluOpType
AX = mybir.AxisListType

D = 1024
SEQ = 4096
CTX = 256
NT = SEQ // 128
NCT = CTX // 128
DIN = 7168
NE = 32
FF = 512
CAP = 1536
GS = 256
import os
CUT = int(os.environ.get('KCUT', '99'))
KB = CAP // 128
EPS = 1e-6
QSCALE = 128.0 ** -0.5
NSLOT = 16
BIG = float(NE * CAP)

C_ID = 0
C_ONES = 128
C_LTRI = 256
C_MRF = 384
C_MKF = 512
C_MRB = 640
C_MKB = 768
C_R1 = 896
C_R2 = 1024
C_COL = 1152
C_ECAP = 1156
NCONST = 1188


def make_consts():
    c = np.zeros((128, NCONST), np.float32)
    i = np.arange(128, dtype=np.float32)
    c[:, C_ID:C_ID + 128] = np.eye(128, dtype=np.float32)
    c[:, C_ONES:C_ONES + 128] = 1.0
    c[:, C_LTRI:C_LTRI + 128] = (i[:, None] < i[None, :]).astype(np.float32)
    m = i[:, None]
    n = i[None, :]
    c[:, C_MRF:C_MRF + 128] = np.maximum(n - m, 0)
    c[:, C_MKF:C_MKF + 128] = (n >= m).astype(np.float32) * QSCALE
    c[:, C_MRB:C_MRB + 128] = np.maximum(m - n, 0)
    c[:, C_MKB:C_MKB + 128] = (m > n).astype(np.float32) * QSCALE
    c[:, C_R1:C_R1 + 128] = (i + 1.0)[None, :]
    c[:, C_R2:C_R2 + 128] = (128.0 - i)[None, :]
    c[:, C_COL + 0] = 127.0 - i
    c[:, C_COL + 1] = i
    c[:, C_COL + 2] = 255.0 - i
    c[:, C_COL + 3] = 128.0 + i
    c[:, C_ECAP:C_ECAP + 32] = (np.arange(32, dtype=np.float32) * CAP)[None, :]
    return c


def make_rot():
    nf = 32
    inv = 10000.0 ** (-np.arange(nf, dtype=np.float32) / nf)
    pos = np.arange(SEQ, dtype=np.float32)
    row = np.floor(pos / 64.0)
    col = pos - row * 64.0
    ang = np.concatenate([row[:, None] * inv, col[:, None] * inv], axis=-1).astype(np.float32)
    cos = np.cos(ang).astype(np.float32)
    sin = np.sin(ang).astype(np.float32)
    rot = np.zeros((NT + 1, 128, 256), np.float32)
    cc = np.repeat(cos, 2, axis=1)
    ss = np.stack([-sin, sin], axis=-1).reshape(SEQ, 128)
    rot[:NT, :, 0:128] = cc.reshape(NT, 128, 128)
    rot[:NT, :, 128:256] = ss.reshape(NT, 128, 128)
    rot[NT, :, 0:128] = 1.0
    return rot


ENG_COMPUTE = ("pe", "act", "dve", "pool")
QUEUES = ("sp", "pool", "act")


CUR = {"st": 0, "on": False, "bank": False}


class Dual:
    def __init__(self, a, b):
        self.v = (a, b)

    def __getitem__(self, idx):
        return self.v[CUR["st"]][idx]


def I(name, *args, **kw):
    return (name, args, kw)


class Prog:
    def __init__(self):
        self.ops = []
        self.lastw = {}
        self.lastr = {}
        self.bar_pending = {e: set() for e in ("pe", "act", "dve", "pool", "sp")}
        self.last_on = {}
        self.dmas_open = []
        self.cur_region = None
        self.regions = []
        self.local = set()
        self.rec = None

    def _k(self, k):
        if not CUR["on"]:
            return k
        if k in self.local:
            return k + "@%d" % CUR["st"]
        if CUR["bank"] and k.startswith("ps") and k[2:].isdigit():
            return "ps%d" % (int(k[2:]) + 4 * CUR["st"])
        return k

    def merge(self, lists, lag):
        items = []
        for s_, l in enumerate(lists):
            for i_, op in enumerate(l):
                items.append((i_ + (lag if s_ == 1 else 0) + 0.25 * s_, s_, i_, op))
        items.sort(key=lambda x: (x[0], x[1]))
        for _, _, _, (eng, fn, r, w, dma) in items:
            self._add(eng, fn, r, w, dma)

    def add(self, eng, fn, r=(), w=(), dma=False):
        r = [self._k(k) for k in r]
        w = [self._k(k) for k in w]
        if self.rec is not None:
            self.rec.append((eng, fn, r, w, dma))
            return None
        return self._add(eng, fn, r, w, dma)

    def _add(self, eng, fn, r=(), w=(), dma=False):
        i = len(self.ops)
        deps = set()
        for k in r:
            deps.update(self.lastw.get(k, {}).values())
        for k in w:
            deps.update(self.lastw.get(k, {}).values())
            deps.update(self.lastr.get(k, {}).values())
        deps.update(self.bar_pending[eng])
        self.bar_pending[eng] = set()
        stream = ("d", i) if dma else eng
        for k in r:
            self.lastr.setdefault(k, {})[stream] = i
        for k in w:
            self.lastw[k] = {stream: i}
            self.lastr[k] = {}
        deps.discard(i)
        self.op_region = getattr(self, "op_region", [])
        self.op_region.append(self.cur_region)
        self.ops.append((eng, fn, sorted(deps), dma))
        if dma:
            self.dmas_open.append(i)
        else:
            self.last_on[eng] = i
        return i

    def begin_region(self, cond_ap, thr, cond_key):
        self.regions.append((cond_ap, thr, sorted(self.lastw.get(cond_key, {}).values())))
        self.cur_region = len(self.regions) - 1

    def end_region(self):
        self.cur_region = None

    def barrier(self):
        deps = set(self.last_on.values()) | set(self.dmas_open)
        self.dmas_open = []
        for e in self.bar_pending:
            self.bar_pending[e] = set(deps) | self.bar_pending[e]

    def emit(self, nc, block, es):
        ops = self.ops
        n = len(ops)
        pos_on = [0] * n
        cpos = {e: 0 for e in ENG_COMPUTE}
        for i, (eng, fn, deps, dma) in enumerate(ops):
            if not dma:
                cpos[eng] += 1
                pos_on[i] = cpos[eng]
        fdeps = []
        has_dep = [False] * n
        for (_, _, wr) in self.regions:
            for j in wr:
                has_dep[j] = True
        last_reg = {e: (0, None) for e in ENG_COMPUTE}
        last_other = {e: 0 for e in ENG_COMPUTE}
        for i, (eng, fn, deps, dma) in enumerate(ops):
            ri = self.op_region[i]
            fl = []
            for j in deps:
                je, _, _, jd = ops[j]
                if not jd and je == eng and not dma:
                    if eng == "pe":
                        continue
                    if eng in ("act", "dve") and pos_on[j] < pos_on[i] - 2:
                        m = last_reg[eng][0] if last_reg[eng][1] != ri else last_other[eng]
                        if m <= pos_on[j]:
                            continue
                fl.append(j)
                has_dep[j] = True
            fdeps.append(fl)
            if not dma and ri is not None:
                if last_reg[eng][1] != ri:
                    last_other[eng] = last_reg[eng][0]
                last_reg[eng] = (pos_on[i], ri)
        sems = {e: es.enter_context(nc.semaphore("s_" + e)) for e in ENG_COMPUTE}
        dsem = {q: [es.enter_context(nc.semaphore("d_%s%d" % (q, s))) for s in range(NSLOT)] for q in QUEUES}
        sig = [None] * n
        cnt = {e: 0 for e in ENG_COMPUTE}
        dcnt = {q: 0 for q in QUEUES}
        prevslot = [None] * n
        for i, (eng, fn, deps, dma) in enumerate(ops):
            if dma:
                k = dcnt[eng]
                dcnt[eng] += 1
                s = k % NSLOT
                sig[i] = (("d", eng, s), 16 * (k // NSLOT + 1))
                if k >= NSLOT:
                    prevslot[i] = (("d", eng, s), 16 * (k // NSLOT))
            else:
                if has_dep[i]:
                    cnt[eng] += 1
                    sig[i] = (("c", eng), cnt[eng])

        def semof(key):
            return sems[key[1]] if key[0] == "c" else dsem[key[1]][key[2]]

        per = {e: [] for e in ("pe", "act", "dve", "pool", "sp")}
        waited = {e: {} for e in per}
        cur_reg = {e: None for e in per}
        snap = {e: None for e in per}
        sigcount = {}
        for i, (eng, fn, deps, dma) in enumerate(ops):
            reg = self.op_region[i]
            if reg != cur_reg[eng]:
                if cur_reg[eng] is not None:
                    waited[eng] = snap[eng]
                condw = []
                if reg is not None:
                    for j in self.regions[reg][2]:
                        key, val = sig[j]
                        if waited[eng].get(key, 0) < val:
                            waited[eng][key] = val
                            condw.append((semof(key), val))
                    snap[eng] = dict(waited[eng])
                cur_reg[eng] = reg
            else:
                condw = []
            waits = {}
            for j in fdeps[i]:
                key, val = sig[j]
                if waits.get(key, 0) < val:
                    waits[key] = val
            if prevslot[i] is not None:
                key, val = prevslot[i]
                if waits.get(key, 0) < val:
                    waits[key] = val
            wl = []
            for key, val in waits.items():
                if waited[eng].get(key, 0) >= val:
                    continue
                waited[eng][key] = val
                wl.append((semof(key), val))
            inc = None
            before = None
            if sig[i] is not None:
                inc = (semof(sig[i][0]), 16 if dma else 1)
                before = (sig[i][0], sig[i][1] - (16 if dma else 1))
            per[eng].append((wl, fn, inc, reg, before, condw))
        final = []
        for q in QUEUES:
            for s in range(NSLOT):
                k = dcnt[q]
                uses = (k - s + NSLOT - 1) // NSLOT if k > s else 0
                if uses > 0:
                    final.append((dsem[q][s], 16 * uses))
        for e in ENG_COMPUTE:
            if cnt[e] > 0:
                final.append((sems[e], cnt[e]))
        self.stats = dict(n_ops=n, cnt=cnt, dcnt=dcnt)

        def mk(eng, with_final):
            def f(e):
                bcreg = None
                if eng == "pool":
                    bcreg = e.alloc_register("bcreg")
                    e.reg_mov(bcreg, NE * CAP - 1)
                creg = e.alloc_register("creg_" + eng) if self.regions else None

                def emit_one(wl, fn, inc):
                    if fn[2].get("bounds_check", None) == "BCREG":
                        fn = (fn[0], fn[1], dict(fn[2], bounds_check=bcreg))
                    for s_, v in wl:
                        e.wait_ge(s_, v)
                    try:
                        ins = getattr(e, fn[0])(*fn[1], **fn[2])
                    except Exception:
                        print("EMIT FAIL", eng, fn[0], {k: str(v)[:200] for k, v in fn[2].items()})
                        raise
                    if inc is not None:
                        ins.then_inc(inc[0], inc[1])

                lst = per[eng]
                k = 0
                while k < len(lst):
                    reg = lst[k][3]
                    if reg is None:
                        emit_one(*lst[k][:3])
                        k += 1
                        continue
                    k2 = k
                    while k2 < len(lst) and lst[k2][3] == reg:
                        k2 += 1
                    grp = lst[k:k2]
                    cond_ap, thr, _ = self.regions[reg]
                    for s_, v in grp[0][5]:
                        e.wait_ge(s_, v)
                    e.reg_load(creg, cond_ap)
                    with e.If_cmp(creg, thr, "IS_GT"):
                        for (wl, fn, inc, _, _, _) in grp:
                            emit_one(wl, fn, inc)
                    comp = {}
                    for (wl, fn, inc, _, before, _) in grp:
                        if inc is None:
                            continue
                        key = before[0]
                        if key not in comp:
                            comp[key] = [before[1], 0, inc[0]]
                        comp[key][1] += inc[1]
                    with e.Else():
                        for key, (bval, tot, semh) in comp.items():
                            if bval > 0:
                                e.wait_ge(semh, bval)
                            e.sem_inc(semh, tot)
                    k = k2
                if with_final:
                    for s_, v in final:
                        e.wait_ge(s_, v)
            return f

        block.sync(mk("sp", True))
        block.scalar(mk("act", False))
        block.vector(mk("dve", False))
        block.gpsimd(mk("pool", False))
        block.tensor(mk("pe", False))


class _Stop(Exception):
    pass


def build(n_layers=2, debug=False, stop_after=None):
    nc = bass.Bass("TRN2", target_bir_lowering=False)
    P = Prog()

    def din(name, shape, dt=F32):
        return nc.dram_tensor(name, list(shape), dt, kind="ExternalInput").ap()

    x_in = din("x", [SEQ, D])
    ctx_in = din("ctx", [CTX, D])
    cvec = din("cvec", [128, 8, 2])
    w_ada = din("w_ada", [2, D, 6 * D])
    b_adaT = din("b_adaT", [2, 128, 48])
    g_mixT = din("g_mixT", [2, 128, 8])
    g_ffnT = din("g_ffnT", [2, 128, 8])
    g_sguT = din("g_sguT", [2, 128, 8])
    g_fin = din("g_final", [1, D])
    w_in = din("w_in", [2, D, DIN])
    w_sT = din("w_sT", [2, 8, 128, 128])
    b_s = din("b_s", [2, 1, D])
    dlog = din("decay_logit", [2, 1, 8])
    w_pa = din("w_pa", [2, D, D])
    w_pb = din("w_pb", [2, D, D])
    w_out = din("w_out", [2, D, D])
    w_r = din("w_r", [2, D, 36])
    b_r = din("b_r", [2, 1, 36])
    w1 = din("w1", [2, NE, D, FF])
    w3 = din("w3", [2, NE, D, FF])
    w2 = din("w2", [2, NE, FF, D])
    consts_d = din("consts", [128, NCONST])
    rot_d = din("rot", [NT + 1, 128, 256])
    out_d = nc.dram_tensor("out", [SEQ, D], F32, kind="ExternalOutput").ap()

    def dscr(name, shape, dt=F32):
        kind = "ExternalOutput" if debug else "Internal"
        return nc.dram_tensor(name, list(shape), dt, kind=kind).ap()

    xs = dscr("xs", [SEQ + CTX, D])
    As = dscr("As", [SEQ + CTX, D], BF16)
    Ts = dscr("Ts", [NT + 1, 128, 1024], BF16)
    Xs = dscr("Xs", [NE * CAP, D], BF16)
    Yall = dscr("Yall", [NE * CAP, D])
    Hs = dscr("Hs", [NT + NCT, 128, 1024], BF16)
    dbgt = dscr("dbgt", [128, 68 + 68 + 1024 + 1024]) if debug else None

    es = ExitStack()
    with es:
        ARENA_WORDS = 52000
        arena = es.enter_context(nc.sbuf_tensor("arena", [128, ARENA_WORDS], F32))
        top = [0]
        peak = [0]

        class Scope:
            def __enter__(self):
                self.m = top[0]
                return self

            def __exit__(self, *a):
                top[0] = self.m
                return False

        def sb(name, shape, dt=F32, stack=None):
            nel = 1
            for d_ in shape[1:]:
                nel *= d_
            words = (nel * (4 if dt in (F32, I32) else 2) + 3) // 4
            words = (words + 7) // 8 * 8
            off = top[0]
            top[0] += words
            peak[0] = max(peak[0], top[0])
            assert top[0] <= ARENA_WORDS, ("SBUF arena overflow", name, top[0])
            ap = arena[:, off:off + words]
            if dt != F32:
                ap = ap.bitcast(dt)
            ap = ap[:, 0:nel]
            if len(shape) == 3:
                ap = ap.rearrange("p (a b) -> p a b", a=shape[1])
            return ap

        psum = es.enter_context(nc.psum_tensor("psum", [128, 4096], F32))

        def pb(b, n=1):
            if CUR["bank"] and CUR["st"] == 1:
                b = b + 4
            return psum[:, 512 * b:512 * (b + n)]

        def sb2(name, shape, dt=F32, stack=None):
            return Dual(sb(name + "a", shape, dt), sb(name + "b", shape, dt))

        def two_stream(tiles, tile_fn, lag_frac=0.5, extra=None):
            lists = [[], []]
            ntile_ops = None
            CUR["on"] = True
            CUR["bank"] = True
            for i_, t_ in enumerate(tiles):
                CUR["st"] = i_ % 2
                P.rec = []
                tile_fn(t_)
                if ntile_ops is None:
                    ntile_ops = len(P.rec)
                lists[i_ % 2].extend(P.rec)
            P.rec = None
            CUR["on"] = False
            CUR["bank"] = False
            CUR["st"] = 0
            if extra:
                lists.append(extra)
            P.merge(lists, int((ntile_ops or 0) * lag_frac))

        def pbk(b, n=1):
            return ["ps%d" % (b + i) for i in range(n)]

        cst = sb("cst", [128, NCONST])
        ident_bf = sb("ident_bf", [128, 128], BF16)
        dl_bc = sb("dl_bc", [128, 8])
        lg = sb("lg", [128, 8])
        cdec = sb("cdec", [128, 8])
        DT = sb("DT", [128, 4, 128])
        qdf = sb("qdf", [128, 4, 128])
        qdb = sb("qdb", [128, 4, 128])
        KD = sb("KD", [128, 4, 8])
        tmpA = sb("tmpA", [128, 128])
        tmpB = sb("tmpB", [128, 128])
        scv = sb("scv", [128, 8, 2])
        modT = sb("modT", [128, 48, 2])
        badaT = sb("badaT", [128, 48])
        gmixT = sb("gmixT", [128, 8])
        gffnT = sb("gffnT", [128, 8])
        gsguT = sb("gsguT", [128, 8])
        a1 = sb("a1", [128, 8, 2])
        a2 = sb("a2", [128, 8, 2])
        small = sb("small", [128, 64])
        rstat = sb("rstat", [128, 8])

        ident = cst[:, C_ID:C_ID + 128]
        ones = cst[:, C_ONES:C_ONES + 128]
        ltri = cst[:, C_LTRI:C_LTRI + 128]

        P.add("sp", I('dma_start', out=cst[:], in_=consts_d), w=["cst"], dma=True)
        P.add("sp", I('dma_start', out=scv[:], in_=cvec), w=["scv"], dma=True)
        P.add("dve", I('tensor_copy', out=ident_bf[:], in_=ident), r=["cst"], w=["identbf"])
        P.add("act", I('activation', out=scv[:], in_=scv[:], func=AF.Silu), r=["scv"], w=["scv"])

        def src_tile(layer, t):
            if layer == 0:
                return x_in[128 * t:128 * (t + 1), :] if t < NT else ctx_in[128 * (t - NT):128 * (t - NT + 1), :]
            return xs[128 * t:128 * (t + 1), :]

        def xs_tile(t):
            return xs[128 * t:128 * (t + 1), :]

        def rms_rstd(xt, xkey, junk, junkkey, col):
            c0 = rstat[:, col:col + 1]
            k = "rstat%d" % col
            P.add("act", I('activation', out=junk, in_=xt, func=AF.Square, accum_out=c0),
                  r=[xkey], w=[junkkey, k])
            P.add("dve", I('tensor_scalar', out=c0, in0=c0, scalar1=1.0 / D, scalar2=EPS,
                                                   op0=ALU.mult, op1=ALU.add), r=[k], w=[k])
            P.add("act", I('activation', out=c0, in_=c0, func=AF.Sqrt), r=[k], w=[k])
            P.add("dve", I('reciprocal', out=c0, in_=c0), r=[k], w=[k])
            return c0, k

        for layer in range(n_layers):
          try:
            L = layer
            has_ctx = (layer == 0)
            tiles_all = list(range(NT)) + ([NT, NT + 1] if has_ctx else [])

            def phase0(L, wst):
                P.add("sp", I('dma_start', out=badaT[:], in_=b_adaT[L]), w=["badaT"], dma=True)
                P.add("sp", I('dma_start', out=gmixT[:], in_=g_mixT[L]), w=["gmixT"], dma=True)
                P.add("sp", I('dma_start', out=gffnT[:], in_=g_ffnT[L]), w=["gffnT"], dma=True)
                P.add("sp", I('dma_start', out=gsguT[:], in_=g_sguT[L]), w=["gsguT"], dma=True)
                P.add("sp", I('dma_start', out=dl_bc[:], in_=dlog[L].partition_broadcast(128)), w=["dl"], dma=True)
                for blk in range(12):
                    wt = wst[blk % 2]
                    wk = "wada%d" % (blk % 2)
                    for kc in range(8):
                        P.add("sp", I('dma_start',
                            out=wt[:, kc, :], in_=w_ada[L, 128 * kc:128 * (kc + 1), 512 * blk:512 * (blk + 1)]),
                            w=[wk + "_%d" % kc], dma=True)
                    for jj in range(4):
                        j = blk * 4 + jj
                        for kc in range(8):
                            P.add("pe", I('matmul',
                                out=pb(0)[:, 2 * j:2 * j + 2], lhsT=wt[:, kc, 128 * jj:128 * (jj + 1)],
                                rhs=scv[:, kc, :], start=(kc == 0), stop=(kc == 7)),
                                r=[wk + "_%d" % kc, "scv"], w=["ps0"])
                mod2 = modT[:].rearrange("p j t -> p (j t)")
                P.add("dve", I('tensor_copy', out=mod2, in_=pb(0)[:, 0:96]), r=["ps0"], w=["modT"])
                for t in range(2):
                    P.add("dve", I('tensor_tensor', out=modT[:, :, t], in0=modT[:, :, t], in1=badaT[:],
                                                                op=ALU.add), r=["modT", "badaT"], w=["modT"])
                for t in range(2):
                    P.add("dve", I('scalar_tensor_tensor',
                        out=a1[:, :, t], in0=modT[:, 8:16, t], scalar=1.0, in1=gmixT[:], op0=ALU.add, op1=ALU.mult),
                        r=["modT", "gmixT"], w=["a1"])
                    P.add("dve", I('scalar_tensor_tensor',
                        out=a2[:, :, t], in0=modT[:, 32:40, t], scalar=1.0, in1=gffnT[:], op0=ALU.add, op1=ALU.mult),
                        r=["modT", "gffnT"], w=["a2"])
                P.add("act", I('activation', out=lg[:], in_=dl_bc[:], func=AF.Exp, scale=-1.0), r=["dl"], w=["lg"])
                P.add("act", I('activation', out=lg[:], in_=lg[:], func=AF.Ln, bias=1.0), r=["lg"], w=["lg"])
                P.add("dve", I('tensor_scalar', out=lg[:], in0=lg[:], scalar1=-1.0, scalar2=None, op0=ALU.mult),
                      r=["lg"], w=["lg"])
                P.add("act", I('activation', out=cdec[:], in_=lg[:], func=AF.Exp, scale=128.0), r=["lg"], w=["cdec"])
                for h in range(4):
                    lf = lg[:, h:h + 1]
                    lb = lg[:, 4 + h:5 + h]
                    P.add("act", I('activation', out=tmpA[:], in_=cst[:, C_MRF:C_MRF + 128], func=AF.Exp, scale=lf),
                          r=["lg", "cst"], w=["tmpA"])
                    P.add("dve", I('tensor_tensor', out=tmpA[:], in0=tmpA[:], in1=cst[:, C_MKF:C_MKF + 128], op=ALU.mult),
                          r=["tmpA", "cst"], w=["tmpA"])
                    P.add("act", I('activation', out=tmpB[:], in_=cst[:, C_MRB:C_MRB + 128], func=AF.Exp, scale=lb),
                          r=["lg", "cst"], w=["tmpB"])
                    P.add("dve", I('tensor_tensor', out=tmpB[:], in0=tmpB[:], in1=cst[:, C_MKB:C_MKB + 128], op=ALU.mult),
                          r=["tmpB", "cst"], w=["tmpB"])
                    P.add("dve", I('tensor_tensor', out=DT[:, h, :], in0=tmpA[:], in1=tmpB[:], op=ALU.add),
                          r=["tmpA", "tmpB"], w=["DT"])
                    P.add("act", I('activation', out=qdf[:, h, :], in_=cst[:, C_R1:C_R1 + 128], func=AF.Exp, scale=lf),
                          r=["lg", "cst"], w=["qdf"])
                    P.add("act", I('activation', out=qdb[:, h, :], in_=cst[:, C_R2:C_R2 + 128], func=AF.Exp, scale=lb),
                          r=["lg", "cst"], w=["qdb"])
                    for cs in range(4):
                        P.add("act", I('activation',
                            out=KD[:, cs, h:h + 1], in_=cst[:, C_COL + cs:C_COL + cs + 1], func=AF.Exp, scale=lf),
                            r=["lg", "cst"], w=["KD"])
                        P.add("act", I('activation',
                            out=KD[:, cs, 4 + h:5 + h], in_=cst[:, C_COL + cs:C_COL + cs + 1], func=AF.Exp, scale=lb),
                            r=["lg", "cst"], w=["KD"])
                kd2 = KD[:].rearrange("p a b -> p (a b)")
                P.add("dve", I('tensor_scalar', out=kd2, in0=kd2, scalar1=QSCALE, scalar2=None, op0=ALU.mult),
                      r=["KD"], w=["KD"])


            if layer == 0:
                P.barrier()
                with Scope() as ph:
                    wst0 = [sb("wada%d" % i, [128, 8, 512], F32, ph) for i in range(2)]
                    phase0(0, wst0)

            def bcast_gate(dst, dkey, which, t):
                for kc in range(8):
                    col = modT[:, which * 8 + kc, t:t + 1]
                    P.add("dve", I('tensor_scalar', out=tmpA[:], in0=ident, scalar1=col, scalar2=None,
                                                                    op0=ALU.mult), r=["modT", "cst"], w=["tmpA"])
                    P.add("pe", I('matmul', out=pb(kc // 4)[:, 128 * (kc % 4):128 * (kc % 4 + 1)],
                                                          lhsT=ones, rhs=tmpA[:], start=True, stop=True),
                          r=["tmpA", "cst"], w=["ps%d" % (kc // 4)])
                P.add("act", I('activation', out=dst, in_=pb(0, 2), func=AF.Identity), r=["ps0", "ps1"], w=[dkey])

            def bcast_feat(dst, dkey, src, skey, t):
                for kc in range(8):
                    col = src[:, kc, t:t + 1]
                    P.add("dve", I('tensor_scalar', out=tmpA[:], in0=ident, scalar1=col, scalar2=None,
                                                                    op0=ALU.mult), r=[skey, "cst"], w=["tmpA"])
                    P.add("pe", I('matmul', out=pb(kc // 4)[:, 128 * (kc % 4):128 * (kc % 4 + 1)],
                                                          lhsT=ones, rhs=tmpA[:], start=True, stop=True),
                          r=["tmpA", "cst"], w=["ps%d" % (kc // 4)])
                P.add("act", I('activation', out=dst, in_=pb(0, 2), func=AF.Identity), r=["ps0", "ps1"], w=[dkey])

            def load_w_cast(dst3, dkey, src2d, ncols):
                for kc in range(src2d.shape[0] // 128):
                    P.add("pool", I('dma_start', out=dst3[:, kc, :], in_=src2d[128 * kc:128 * (kc + 1), :]),
                          w=[dkey], dma=True)

            def h_tile(ph_bufs, t, layer_src, a_ap, b_ap, cond):
                xt, xnb, hT, junk = ph_bufs
                P.add("sp", I('dma_start', out=xt[:], in_=layer_src), w=["xt"], dma=True)
                c0, k = rms_rstd(xt[:], "xt", junk[:], "junk", 2 + CUR["st"] if CUR["on"] else 0)
                P.add("dve", I('tensor_scalar', out=xnb[:], in0=xt[:], scalar1=c0, scalar2=None, op0=ALU.mult),
                      r=["xt", k], w=["xnb"])
                trp = pb(0).bitcast(BF16)
                for kc in range(8):
                    P.add("pe", I('transpose', out=trp[:, 128 * kc:128 * (kc + 1)],
                                                             in_=xnb[:, 128 * kc:128 * (kc + 1)], identity=ident_bf[:]),
                          r=["xnb", "identbf"], w=["ps0"])
                for kc in range(8):
                    P.add("act", I('activation', out=hT[:, kc, :], in_=trp[:, 128 * kc:128 * (kc + 1)],
                                                               func=AF.Identity, scale=a_ap[:, kc, cond:cond + 1],
                                                               bias=b_ap[:, kc, cond:cond + 1]),
                          r=["ps0", "a1", "a2", "modT"], w=["hT"])

            def proj_tok(hT, wt, wkey, col0, ncol, bank):
                for nb in range(ncol // 512):
                    for kc in range(8):
                        P.add("pe", I('matmul',
                            out=pb(bank + nb), lhsT=hT[:, kc, :], rhs=wt[:, kc, col0 + 512 * nb:col0 + 512 * (nb + 1)],
                            start=(kc == 0), stop=(kc == 7)), r=["hT", wkey], w=["ps%d" % (bank + nb)])

            P.barrier()
            with Scope() as ph:
                wA = sb("wA", [128, 8, 3072], BF16, ph)
                wpa = sb("wpa", [128, 8, 1024], BF16, ph)
                wsT = sb("wsT", [128, 8, 128], BF16, ph)
                bs_bc = sb("bs_bc", [128, 8, 128], F32, ph)
                xt = sb2("xt", [128, D], F32, ph)
                xnb = sb2("xnb", [128, D], BF16, ph)
                hT = sb2("hT", [128, 8, 128], BF16, ph)
                junk = sb2("junk", [128, D], BF16, ph)
                guT = sb2("guT", [128, 8, 128], BF16, ph)
                gv = sb2("gv", [128, D], F32, ph)
                vhat = sb2("vhat", [128, D], BF16, ph)
                bst = sb2("bst", [128, 2, 6], F32, ph)
                mv = sb2("mv", [128, 2], F32, ph)
                yaT = sb2("yaT", [128, 8, 128], BF16, ph)
                sga = sb2("sga", [128, D], F32, ph)
                ao = sb2("ao", [128, D], BF16, ph)
                P.local = {"xt", "xnb", "hT", "junk", "guT", "gv", "vhat", "bst", "mv", "yaT", "sga", "ao",
                           "rstat2", "rstat3"}
                load_w_cast(wA[:, :, 0:2048], "wA", w_in[L][:, 0:2048], 2048)
                load_w_cast(wA[:, :, 2048:3072], "wA", w_in[L][:, 5120:6144], 1024)
                load_w_cast(wpa, "wpa", w_pa[L], 1024)
                for g in range(8):
                    P.add("pool", I('dma_start', out=wsT[:, g, :], in_=w_sT[L, g]), w=["wsT"], dma=True)
                P.add("sp", I('dma_start', out=bs_bc[:].rearrange("p g q -> p (g q)"), in_=b_s[L].partition_broadcast(128)),
                      w=["bs_bc"], dma=True)
                def tileA(t):
                    cond = 0 if t < NT else 1
                    h_tile((xt, xnb, hT, junk), t, src_tile(layer, t), a1, modT[:, 0:8, :], cond)
                    P.add("pool", I('dma_start', out=Hs[t], in_=hT[:].rearrange("p a b -> p (a b)")), r=["hT"], w=["Hs%d" % t], dma=True)
                    if t >= NT and not has_ctx:
                        return
                    for fc in range(8):
                        for kc in range(8):
                            P.add("pe", I('matmul',
                                out=pb(fc // 4)[:, 128 * (fc % 4):128 * (fc % 4 + 1)],
                                lhsT=wA[:, kc, 128 * fc:128 * (fc + 1)], rhs=hT[:, kc, :],
                                start=(kc == 0), stop=(kc == 7)), r=["hT", "wA"], w=["ps%d" % (fc // 4)])
                    P.add("act", I('activation', out=guT[:].rearrange("p a b -> p (a b)"), in_=pb(0, 2), func=AF.Gelu_apprx_tanh),
                          r=["ps0", "ps1"], w=["guT"])
                    proj_tok(hT, wA, "wA", 1024, 1024, 2)
                    P.add("act", I('activation', out=gv[:], in_=pb(2, 2), func=AF.Gelu_apprx_tanh),
                          r=["ps2", "ps3"], w=["gv"])
                    for hh in range(2):
                        P.add("dve", I('bn_stats', out=bst[:, hh, :], in_=gv[:, 512 * hh:512 * (hh + 1)]),
                              r=["gv"], w=["bst"])
                    P.add("dve", I('bn_aggr', out=mv[:], in_=bst[:].rearrange("p a b -> p (a b)")), r=["bst"], w=["mv"])
                    P.add("dve", I('tensor_scalar', out=mv[:, 1:2], in0=mv[:, 1:2], scalar1=EPS, scalar2=None, op0=ALU.add),
                          r=["mv"], w=["mv"])
                    P.add("act", I('activation', out=mv[:, 1:2], in_=mv[:, 1:2], func=AF.Sqrt), r=["mv"], w=["mv"])
                    P.add("dve", I('reciprocal', out=mv[:, 1:2], in_=mv[:, 1:2]), r=["mv"], w=["mv"])
                    P.add("dve", I('tensor_scalar', out=vhat[:], in0=gv[:], scalar1=mv[:, 0:1], scalar2=mv[:, 1:2],
                                                           op0=ALU.subtract, op1=ALU.mult), r=["gv", "mv"], w=["vhat"])
                    for g in range(8):
                        P.add("pe", I('matmul', out=pb(g // 4)[:, 128 * (g % 4):128 * (g % 4 + 1)],
                                                            lhsT=vhat[:, 128 * g:128 * (g + 1)], rhs=wsT[:, g, :],
                                                            start=True, stop=True), r=["vhat", "wsT"], w=["ps%d" % (g // 4)])
                    for g in range(8):
                        P.add("dve", I('scalar_tensor_tensor',
                            out=gv[:, 128 * g:128 * (g + 1)], in0=pb(g // 4)[:, 128 * (g % 4):128 * (g % 4 + 1)],
                            scalar=gsguT[:, g:g + 1], in1=bs_bc[:, g, :], op0=ALU.mult, op1=ALU.add),
                            r=["ps%d" % (g // 4), "gsguT", "bs_bc", "vhat"], w=["gv"])
                    P.add("dve", I('tensor_tensor', out=yaT[:].rearrange("p a b -> p (a b)"), in0=gv[:],
                                                           in1=guT[:].rearrange("p a b -> p (a b)"), op=ALU.mult),
                          r=["gv", "guT"], w=["yaT"])
                    for nb in range(2):
                        for kc in range(8):
                            P.add("pe", I('matmul',
                                out=pb(2 + nb), lhsT=yaT[:, kc, :], rhs=wpa[:, kc, 512 * nb:512 * (nb + 1)],
                                start=(kc == 0), stop=(kc == 7)), r=["yaT", "wpa"], w=["ps%d" % (2 + nb)])
                    proj_tok(hT, wA, "wA", 2048, 1024, 0)
                    P.add("act", I('activation', out=sga[:], in_=pb(0, 2), func=AF.Sigmoid), r=["ps0", "ps1"], w=["sga"])
                    P.add("dve", I('tensor_tensor', out=ao[:], in0=pb(2, 2), in1=sga[:], op=ALU.mult),
                          r=["ps2", "ps3", "sga"], w=["ao"])
                    P.add("pool", I('dma_start', out=As[128 * t:128 * (t + 1), :], in_=ao[:]),
                          r=["ao"], w=["As%d" % t], dma=True)

                two_stream(tiles_all, tileA)
                if not has_ctx:
                    P.barrier()
                    for t_ in (NT, NT + 1):
                        tileA(t_)

            if stop_after == ('A', layer):
                raise _Stop
            P.barrier()
            with Scope() as ph:
                wB = sb("wB", [128, 8, 4096], BF16, ph)
                wpb = sb("wpb", [128, 8, 1024], BF16, ph)
                wout = sb("wout", [128, 8, 1024], BF16, ph)
                gt1_bc = [sb("gt1bc%d" % i, [128, D], F32, ph) for i in range(2 if has_ctx else 1)]
                xt = sb2("xt", [128, D], F32, ph)
                hT = sb2("hT", [128, 8, 128], BF16, ph)
                rt = sb2("rt", [128, 256], F32, ph)
                t1 = sb("t1", [128, D], F32, ph)
                t2 = sb("t2", [128, D], F32, ph)
                t1h = Dual(t1[:, 0:512], t1[:, 512:1024])
                t1d = Dual(t1, sb("t1x", [128, D], F32, ph))
                t2h = Dual(t2[:, 0:512], t2[:, 512:1024])
                qkr = sb2("qkr", [128, 8, 128], BF16, ph)
                qT = sb2("qT", [128, 4, 128], BF16, ph)
                qTf = sb2("qTf", [128, 4, 128], BF16, ph)
                qTb = sb2("qTb", [128, 4, 128], BF16, ph)
                kT = sb2("kT", [128, 4, 128], BF16, ph)
                kd = sb2("kd", [128, 4, 128], BF16, ph)
                vb = sb2("vb", [128, 4, 256], BF16, ph)
                sgr = sb2("sgr", [128, D], BF16, ph)
                sgb = sb2("sgb", [128, D], BF16, ph)
                PT = sb2("PT", [128, 4, 128], BF16, ph)
                yb = sb2("yb", [128, D], BF16, ph)
                tT = sb2("tT", [128, 8, 128], BF16, ph)
                apt = sb2("apt", [128, D], BF16, ph)
                mg = sb2("mg", [128, D], BF16, ph)
                Sfb = sb("Sfb", [128, 4, 256], BF16, ph)
                Tb = sb("Tb", [128, 4, 256], F32, ph)
                Sf = Tb
                Tbb = sb2("Tbb", [128, 4, 256], BF16, ph)
                scf = sb("scf", [128, 4, 256], F32, ph)
                lnst = sb2("lnst", [128, 4, 6], F32, ph)
                lnmv = sb2("lnmv", [128, 4, 2], F32, ph)
                lnb = sb("lnb", [128, 4], F32, ph)

                load_w_cast(wB[:, :, 0:3072], "wB", w_in[L][:, 2048:5120], 3072)
                load_w_cast(wB[:, :, 3072:4096], "wB", w_in[L][:, 6144:7168], 1024)
                load_w_cast(wpb, "wpb", w_pb[L], 1024)
                load_w_cast(wout, "wout", w_out[L], 1024)
                for i in range(len(gt1_bc)):
                    bcast_gate(gt1_bc[i][:], "gt1bc%d" % i, 2, i)

                def state_update(S, skey, kdcol_set, coff, vkey="vb", kvb=6):
                    for h in range(4):
                        P.add("dve", I('tensor_scalar', out=kd[:, h, :], in0=qkr[:, 4 + h, :],
                                                                    scalar1=KD[:, kdcol_set, coff + h:coff + h + 1], scalar2=None,
                                                                    op0=ALU.mult), r=["qkr", "KD"], w=["kd"])
                    for h in range(4):
                        P.add("pe", I('matmul', out=pb(kvb + h // 2)[:, 256 * (h % 2):256 * (h % 2 + 1)],
                                                            lhsT=kd[:, h, :], rhs=vb[:, h, :], start=True, stop=True),
                              r=["kd", vkey], w=["ps%d" % (kvb + h // 2)])
                    if S is not None:
                        for h in range(4):
                            P.add("dve", I('scalar_tensor_tensor',
                                out=S[:, h, :], in0=S[:, h, :], scalar=cdec[:, coff + h:coff + h + 1],
                                in1=pb(kvb + h // 2)[:, 256 * (h % 2):256 * (h % 2 + 1)], op0=ALU.mult, op1=ALU.add),
                                r=[skey, "cdec", "ps%d" % (kvb + h // 2)], w=[skey])

                def rotary(ps_ap, nh, h0, rot_idx, pskeys=("ps1",), t1=t1, t2=t2):
                    pskeys = list(pskeys)
                    P.add("sp", I('dma_start', out=rt[:], in_=rot_d[rot_idx]), w=["rt"], dma=True)
                    w = nh * 128
                    src3 = ps_ap.rearrange("p (h j two) -> p h j two", h=nh, two=2)
                    t2v = t2[:, 0:w].rearrange("p (h j two) -> p h j two", h=nh, two=2)
                    Sv = rt[:, 128:256].rearrange("p (j two) -> p j two", two=2)
                    for hh in range(nh):
                        P.add("dve", I('tensor_tensor', out=t1[:, 128 * hh:128 * (hh + 1)],
                                                                      in0=ps_ap[:, 128 * hh:128 * (hh + 1)], in1=rt[:, 0:128], op=ALU.mult),
                              r=pskeys + ["rt"], w=["t1"])
                        P.add("dve", I('tensor_tensor', out=t2v[:, hh, :, 0], in0=src3[:, hh, :, 1], in1=Sv[:, :, 0], op=ALU.mult),
                              r=pskeys + ["rt"], w=["t2"])
                        P.add("dve", I('tensor_tensor', out=t2v[:, hh, :, 1], in0=src3[:, hh, :, 0], in1=Sv[:, :, 1], op=ALU.mult),
                              r=pskeys + ["rt"], w=["t2"])
                    P.add("dve", I('tensor_tensor', out=qkr[:, h0:h0 + nh, :].rearrange("p a b -> p (a b)"),
                                                            in0=t1[:, 0:w], in1=t2[:, 0:w], op=ALU.add),
                          r=["t1", "t2"], w=["qkr"])

                def load_hT(t):
                    P.add("sp", I('dma_start', out=hT[:].rearrange("p a b -> p (a b)"), in_=Hs[t]), r=["Hs%d" % t], w=["hT"], dma=True)

                def kv_tile(t, cond, rot_idx, kb=1, vbk=2):
                    load_hT(t)
                    proj_tok(hT, wB, "wB", 512, 512, kb)
                    proj_tok(hT, wB, "wB", 1024, 1024, vbk)
                    P.add("act", I('activation', out=vb[:].rearrange("p a b -> p (a b)"), in_=pb(vbk, 2), func=AF.Identity),
                          r=["ps%d" % vbk, "ps%d" % (vbk + 1)], w=["vb"])
                    rotary(pb(kb), 4, 4, rot_idx, ("ps%d" % kb,), t1=t1h, t2=t2h)

                def store_state_bf(S, skey, dst_idx):
                    P.add("act", I('activation', out=Tbb[:].rearrange("p a b -> p (a b)"), in_=S[:].rearrange("p a b -> p (a b)"),
                                                        func=AF.Identity), r=[skey], w=["Tbb"])
                    P.add("pool", I('dma_start', out=Ts[dst_idx], in_=Tbb[:].rearrange("p a b -> p (a b)")),
                          r=["Tbb"], w=["Ts%d" % dst_idx], dma=True)

                for j in range(NCT):
                    kv_tile(NT + j, 1, NT)
                    fset = 2 if j == 0 else 0
                    bset = 1 if j == 0 else 3
                    state_update(None, None, fset, 0)
                    if j == 0:
                        P.add("act", I('activation', out=scf[:].rearrange("p a b -> p (a b)"), in_=pb(6, 2), func=AF.Identity),
                              r=["ps6", "ps7"], w=["scf"])
                    else:
                        P.add("dve", I('tensor_tensor', out=scf[:].rearrange("p a b -> p (a b)"), in0=scf[:].rearrange("p a b -> p (a b)"),
                                                               in1=pb(6, 2), op=ALU.add), r=["scf", "ps6", "ps7"], w=["scf"])
                    state_update(None, None, bset, 4)
                    if j == 0:
                        P.add("act", I('activation', out=Tb[:].rearrange("p a b -> p (a b)"), in_=pb(6, 2), func=AF.Identity),
                              r=["ps6", "ps7"], w=["Tb"])
                    else:
                        P.add("dve", I('tensor_tensor', out=Tb[:].rearrange("p a b -> p (a b)"), in0=Tb[:].rearrange("p a b -> p (a b)"),
                                                               in1=pb(6, 2), op=ALU.add), r=["Tb", "ps6", "ps7"], w=["Tb"])
                        if has_ctx:
                            state_update(None, None, 1, 4)
                            P.add("act", I('activation', out=Tbb[:].rearrange("p a b -> p (a b)"), in_=pb(6, 2), func=AF.Identity),
                                  r=["ps6", "ps7"], w=["Tbb"])
                            P.add("pool", I('dma_start', out=Ts[NT], in_=Tbb[:].rearrange("p a b -> p (a b)")),
                                  r=["Tbb"], w=["Ts%d" % NT], dma=True)
                store_state_bf(Tb, "Tb", NT - 1)
                def b1_tile(c):
                    kv_tile(c, 0, c, kb=0, vbk=1)
                    state_update(Tb, "Tb", 1, 4, kvb=2)
                    store_state_bf(Tb, "Tb", c - 1)

                P.local = {"hT", "rt", "t1", "t2", "qkr", "kd", "vb", "Tbb"}
                P.barrier()
                two_stream(list(range(NT - 1, 0, -1)), b1_tile)
                P.barrier()

                if stop_after == ('B1', layer):
                    raise _Stop
                def fwd_tile(t, cond, rot_idx, first, zero_b):
                    gbc = gt1_bc[cond]
                    gkey = "gt1bc%d" % cond
                    P.add("sp", I('dma_start', out=xt[:], in_=src_tile(layer, t)), w=["xt"], dma=True)
                    load_hT(t)
                    if not zero_b:
                        P.add("sp", I('dma_start', out=Tbb[:].rearrange("p a b -> p (a b)"), in_=Ts[t if t < NT else NT]),
                              r=["Ts%d" % (t if t < NT else NT)], w=["Tbb"], dma=True)
                    P.add("sp", I('dma_start', out=apt[:], in_=As[128 * t:128 * (t + 1), :]), r=["As%d" % t], w=["apt"], dma=True)
                    proj_tok(hT, wB, "wB", 0, 512, 0)
                    proj_tok(hT, wB, "wB", 512, 512, 1)
                    proj_tok(hT, wB, "wB", 1024, 1024, 2)
                    P.add("act", I('activation', out=vb[:].rearrange("p a b -> p (a b)"), in_=pb(2, 2), func=AF.Identity),
                          r=["ps2", "ps3"], w=["vb"])
                    rotary(pb(0), 4, 0, rot_idx, ("ps0",), t1=t1d, t2=t2h)
                    rotary(pb(1), 4, 4, rot_idx, ("ps1",), t1=t1d, t2=t2h)
                    trp = pb(2).bitcast(BF16)
                    trpB = pb(0).bitcast(BF16)
                    trpC = pb(1).bitcast(BF16)
                    for i8 in range(8):
                        P.add("pe", I('transpose', out=trp[:, 128 * i8:128 * (i8 + 1)], in_=qkr[:, i8, :], identity=ident_bf[:]),
                              r=["qkr", "identbf"], w=["ps2"])
                    P.add("act", I('activation', out=qT[:].rearrange("p a b -> p (a b)"), in_=trp[:, 0:512], func=AF.Identity),
                          r=["ps2"], w=["qT"])
                    P.add("act", I('activation', out=kT[:].rearrange("p a b -> p (a b)"), in_=trp[:, 512:1024], func=AF.Identity),
                          r=["ps2"], w=["kT"])
                    P.add("dve", I('tensor_tensor', out=qTf[:].rearrange("p a b -> p (a b)"), in0=qT[:].rearrange("p a b -> p (a b)"),
                                   in1=qdf[:].rearrange("p a b -> p (a b)"), op=ALU.mult), r=["qT", "qdf"], w=["qTf"])
                    P.add("dve", I('tensor_tensor', out=qTb[:].rearrange("p a b -> p (a b)"), in0=qT[:].rearrange("p a b -> p (a b)"),
                                   in1=qdb[:].rearrange("p a b -> p (a b)"), op=ALU.mult), r=["qT", "qdb"], w=["qTb"])
                    for h in range(4):
                        P.add("pe", I('matmul', out=pb(3)[:, 128 * h:128 * (h + 1)], lhsT=kT[:, h, :], rhs=qT[:, h, :],
                                                            start=True, stop=True), r=["kT", "qT"], w=["ps3"])
                    P.add("dve", I('tensor_tensor', out=PT[:].rearrange("p a b -> p (a b)"), in0=pb(3),
                                                           in1=DT[:].rearrange("p a b -> p (a b)"), op=ALU.mult), r=["ps3", "DT"], w=["PT"])
                    if CUT <= 1:
                        return
                    proj_tok(hT, wB, "wB", 2048, 1024, 0)
                    P.add("act", I('activation', out=sgr[:], in_=pb(0, 2), func=AF.Silu), r=["ps0", "ps1"], w=["sgr"])
                    for h in range(4):
                        oap = pb(2 + h // 2)[:, 256 * (h % 2):256 * (h % 2 + 1)]
                        okey = "ps%d" % (2 + h // 2)
                        nterm = 1 + (0 if first else 1) + (0 if zero_b else 1)
                        P.add("pe", I('matmul', out=oap, lhsT=PT[:, h, :], rhs=vb[:, h, :], start=True, stop=(nterm == 1)),
                              r=["PT", "vb"], w=[okey])
                        done = 1
                        if not first:
                            done += 1
                            P.add("pe", I('matmul', out=oap, lhsT=qTf[:, h, :], rhs=Sfb[:, h, :], start=False, stop=(done == nterm)),
                                  r=["qTf", "Sfb"], w=[okey])
                        if not zero_b:
                            done += 1
                            P.add("pe", I('matmul', out=oap, lhsT=qTb[:, h, :], rhs=Tbb[:, h, :], start=False, stop=True),
                                  r=["qTb", "Tbb"], w=[okey])
                    if CUT <= 2:
                        return
                    P.add("act", I('activation', out=t1d[:], in_=pb(2, 2), func=AF.Identity), r=["ps2", "ps3"], w=["t1"])
                    for h in range(4):
                        P.add("dve", I('bn_stats', out=lnst[:, h, :], in_=t1d[:, 256 * h:256 * (h + 1)]), r=["t1"], w=["lnst"])
                    for h in range(4):
                        P.add("dve", I('bn_aggr', out=lnmv[:, h, :], in_=lnst[:, h, :]), r=["lnst"], w=["lnmv"])
                    P.add("dve", I('tensor_scalar', out=lnmv[:, :, 1], in0=lnmv[:, :, 1], scalar1=EPS, scalar2=None, op0=ALU.add),
                          r=["lnmv"], w=["lnmv"])
                    P.add("act", I('activation', out=lnmv[:, :, 1], in_=lnmv[:, :, 1], func=AF.Sqrt), r=["lnmv"], w=["lnmv"])
                    P.add("dve", I('reciprocal', out=lnmv[:, :, 1], in_=lnmv[:, :, 1]), r=["lnmv"], w=["lnmv"])
                    for h in range(4):
                        P.add("dve", I('tensor_scalar', out=t1d[:, 256 * h:256 * (h + 1)], in0=t1d[:, 256 * h:256 * (h + 1)], scalar1=lnmv[:, h, 0:1],
                                       scalar2=lnmv[:, h, 1:2], op0=ALU.subtract, op1=ALU.mult),
                              r=["t1", "lnmv"], w=["t1"])
                    P.add("dve", I('tensor_tensor', out=yb[:], in0=t1d[:], in1=sgr[:], op=ALU.mult), r=["t1", "sgr"], w=["yb"])
                    if CUT <= 3:
                        return
                    state_update(Sf, "Sf", 0, 0, kvb=0)
                    P.add("act", I('activation', out=Sfb[:].rearrange("p a b -> p (a b)"), in_=Sf[:].rearrange("p a b -> p (a b)"),
                                                        func=AF.Identity), r=["Sf"], w=["Sfb"])
                    if CUT <= 4:
                        return
                    proj_tok(hT, wB, "wB", 3072, 1024, 2)
                    P.add("act", I('activation', out=sgb[:], in_=pb(2, 2), func=AF.Sigmoid), r=["ps2", "ps3"], w=["sgb"])
                    for kc in range(8):
                        P.add("pe", I('transpose', out=trpB[:, 128 * kc:128 * (kc + 1)], in_=yb[:, 128 * kc:128 * (kc + 1)], identity=ident_bf[:]),
                              r=["yb", "identbf"], w=["ps0"])
                    P.add("act", I('activation', out=tT[:].rearrange("p a b -> p (a b)"), in_=trpB, func=AF.Identity), r=["ps0"], w=["tT"])
                    for nb in range(2):
                        for kc in range(8):
                            P.add("pe", I('matmul', out=pb(2 + nb), lhsT=tT[:, kc, :], rhs=wpb[:, kc, 512 * nb:512 * (nb + 1)],
                                                                        start=(kc == 0), stop=(kc == 7)), r=["tT", "wpb"], w=["ps%d" % (2 + nb)])
                    P.add("dve", I('tensor_tensor', out=t1d[:], in0=pb(2, 2), in1=sgb[:], op=ALU.mult), r=["ps2", "ps3", "sgb"], w=["t1"])
                    P.add("dve", I('tensor_tensor', out=mg[:], in0=t1d[:], in1=apt[:], op=ALU.add), r=["t1", "apt"], w=["mg"])
                    if CUT <= 5:
                        return
                    for kc in range(8):
                        P.add("pe", I('transpose', out=trpC[:, 128 * kc:128 * (kc + 1)], in_=mg[:, 128 * kc:128 * (kc + 1)], identity=ident_bf[:]),
                              r=["mg", "identbf"], w=["ps1"])
                    P.add("act", I('activation', out=tT[:].rearrange("p a b -> p (a b)"), in_=trpC, func=AF.Identity), r=["ps1"], w=["tT"])
                    for nb in range(2):
                        for kc in range(8):
                            P.add("pe", I('matmul', out=pb(nb), lhsT=tT[:, kc, :], rhs=wout[:, kc, 512 * nb:512 * (nb + 1)],
                                                                        start=(kc == 0), stop=(kc == 7)), r=["tT", "wout"], w=["ps%d" % nb])
                    P.add("dve", I('tensor_tensor', out=t1d[:], in0=pb(0, 2), in1=gbc[:], op=ALU.mult), r=["ps0", "ps1", gkey], w=["t1"])
                    P.add("dve", I('tensor_tensor', out=t1d[:], in0=t1d[:], in1=xt[:], op=ALU.add), r=["t1", "xt"], w=["t1"])
                    P.add("pool", I('dma_start', out=xs_tile(t), in_=t1d[:]), r=["t1"], w=["xs%d" % t], dma=True)

                if has_ctx:
                    P.add("dve", I('memset', Sf[:].rearrange("p a b -> p (a b)"), 0.0), w=["Sf"])
                    fwd_tile(NT, 1, NT, True, False)
                    fwd_tile(NT + 1, 1, NT, False, True)
                P.add("dve", I('tensor_copy', out=Sf[:].rearrange("p a b -> p (a b)"), in_=scf[:].rearrange("p a b -> p (a b)")),
                      r=["scf"], w=["Sf"])
                P.add("act", I('activation', out=Sfb[:].rearrange("p a b -> p (a b)"), in_=Sf[:].rearrange("p a b -> p (a b)"),
                                                    func=AF.Identity), r=["Sf"], w=["Sfb"])
                P.local = {"xt", "hT", "apt", "Tbb", "rt", "t1", "t2", "qkr", "qT", "qTf", "qTb", "kT", "kd", "vb", "sgr", "sgb",
                           "PT", "yb", "tT", "mg", "lnst", "lnmv"}
                P.barrier()
                two_stream(list(range(NT)), lambda c: fwd_tile(c, 0, c, False, False))

            if stop_after == ('B', layer):
                raise _Stop
            P.barrier()
            with Scope() as ph:
                slots_i = sb("slots_i", [128, 68], I32, ph)
                wts = sb("wts", [128, 34, 2], F32, ph)
                counts_i = sb("counts_i", [128, 32], I32, ph)
                gt2_bc = [sb("gt2bc%d" % i, [128, D], F32, ph) for i in range(2 if has_ctx else 1)]
                with Scope() as ph1:
                    A2bc = [sb("A2bc%d" % i, [128, D], F32, ph1) for i in range(2 if has_ctx else 1)]
                    B2bc = [sb("B2bc%d" % i, [128, D], F32, ph1) for i in range(2 if has_ctx else 1)]
                    wr = sb("wr", [128, 8, 36], F32, ph1)
                    br_bc = sb("br_bc", [128, 36], F32, ph1)
                    xt = sb2("xt", [128, D], F32, ph1)
                    junk = sb2("junk", [128, D], BF16, ph1)
                    h2f = sb2("h2f", [128, D], F32, ph1)
                    h2b = sb2("h2b", [128, D], BF16, ph1)
                    h2T = sb2("h2T", [128, 8, 128], F32, ph1)
                    lgt = sb2("lgt", [128, 36], F32, ph1)
                    msk = sb2("msk", [128, 32], F32, ph1)
                    sel = sb2("sel", [128, 32], F32, ph1)
                    oh1 = sb2("oh1", [128, 32], F32, ph1)
                    oh2 = sb2("oh2", [128, 32], F32, ph1)
                    posm = sb2("posm", [128, 32], F32, ph1)
                    ovr = sb2("ovr", [128, 32], F32, ph1)
                    run_bc = sb("run_bc", [128, 32], F32, ph1)
                    mx8 = sb2("mx8", [128, 8], F32, ph1)
                    sm = sb2("sm", [128, 16], F32, ph1)
                    for i in range(len(gt2_bc)):
                        bcast_gate(gt2_bc[i][:], "gt2bc%d" % i, 5, i)
                        bcast_feat(A2bc[i][:], "A2bc%d" % i, a2, "a2", i)
                        bcast_gate(B2bc[i][:], "B2bc%d" % i, 3, i)
                    for kc in range(8):
                        P.add("sp", I('dma_start', out=wr[:, kc, :], in_=w_r[L, 128 * kc:128 * (kc + 1), :]), w=["wr"], dma=True)
                    P.add("sp", I('dma_start', out=br_bc[:], in_=b_r[L].partition_broadcast(128)), w=["br_bc"], dma=True)
                    P.add("dve", I('memset', run_bc[:], 0.0), w=["run_bc"])
                    def tileM1(t):
                        cond = 0 if t < NT else 1
                        P.add("sp", I('dma_start', out=xt[:], in_=xs_tile(t)), r=["xs%d" % t], w=["xt"], dma=True)
                        c0, k = rms_rstd(xt[:], "xt", junk[:], "junk", 2 + CUR["st"])
                        P.add("dve", I('scalar_tensor_tensor', out=h2f[:], in0=xt[:], scalar=c0, in1=A2bc[cond][:],
                                                                                 op0=ALU.mult, op1=ALU.mult), r=["xt", k, "A2bc%d" % cond], w=["h2f"])
                        P.add("dve", I('tensor_tensor', out=h2f[:], in0=h2f[:], in1=B2bc[cond][:], op=ALU.add),
                              r=["h2f", "B2bc%d" % cond], w=["h2f"])
                        P.add("act", I('activation', out=h2b[:], in_=h2f[:], func=AF.Identity), r=["h2f"], w=["h2b"])
                        for kc in range(8):
                            P.add("pe", I('transpose', out=pb(kc // 4)[:, 128 * (kc % 4):128 * (kc % 4 + 1)],
                                                                     in_=h2f[:, 128 * kc:128 * (kc + 1)], identity=ident),
                                  r=["h2f", "cst"], w=["ps%d" % (kc // 4)])
                        P.add("act", I('activation', out=h2T[:].rearrange("p a b -> p (a b)"), in_=pb(0, 2), func=AF.Identity),
                              r=["ps0", "ps1"], w=["h2T"])
                        for kc in range(8):
                            P.add("pe", I('matmul', out=pb(2)[:, 0:36], lhsT=h2T[:, kc, :], rhs=wr[:, kc, :],
                                                                  start=(kc == 0), stop=(kc == 7)), r=["h2T", "wr"], w=["ps2"])
                        P.add("dve", I('tensor_tensor', out=lgt[:], in0=pb(2)[:, 0:36], in1=br_bc[:], op=ALU.add),
                              r=["ps2", "br_bc"], w=["lgt"])
                        gmax = sm[:, 0:1]
                        P.add("dve", I('reduce_max', out=gmax, in_=lgt[:, 0:4], axis=AX.X), r=["lgt"], w=["sm0"])
                        P.add("dve", I('tensor_scalar', out=sm[:, 4:8], in0=lgt[:, 0:4], scalar1=gmax, scalar2=None, op0=ALU.is_ge),
                              r=["lgt", "sm0"], w=["goh"])
                        P.add("dve", I('tensor_scalar', out=sm[:, 1:2], in0=gmax, scalar1=-1.0, scalar2=None, op0=ALU.mult),
                              r=["sm0"], w=["sm1"])
                        P.add("act", I('activation', out=sm[:, 8:12], in_=lgt[:, 0:4], func=AF.Exp, bias=sm[:, 1:2], accum_out=sm[:, 2:3]),
                              r=["lgt", "sm1"], w=["sm2", "gexp"])
                        P.add("dve", I('reciprocal', out=sm[:, 2:3], in_=sm[:, 2:3]), r=["sm2"], w=["sm2"])
                        P.add("dve", I('tensor_scalar', out=sm[:, 4:8], in0=sm[:, 4:8], scalar1=1e30, scalar2=-1e30, op0=ALU.mult, op1=ALU.add),
                              r=["goh"], w=["goh"])
                        for g in range(4):
                            P.add("dve", I('tensor_scalar', out=msk[:, 8 * g:8 * (g + 1)], in0=lgt[:, 4 + 8 * g:12 + 8 * g],
                                                                        scalar1=sm[:, 4 + g:5 + g], scalar2=None, op0=ALU.add),
                                  r=["lgt", "goh"], w=["msk"])
                        P.add("dve", I('max', out=mx8[:], in_=msk[:]), r=["msk"], w=["mx8"])
                        P.add("dve", I('tensor_scalar', out=oh1[:], in0=msk[:], scalar1=mx8[:, 0:1], scalar2=None, op0=ALU.is_ge),
                              r=["msk", "mx8"], w=["oh1"])
                        P.add("dve", I('tensor_scalar', out=sel[:], in0=msk[:], scalar1=mx8[:, 1:2], scalar2=None, op0=ALU.is_ge),
                              r=["msk", "mx8"], w=["sel"])
                        P.add("dve", I('tensor_tensor', out=oh2[:], in0=sel[:], in1=oh1[:], op=ALU.subtract), r=["sel", "oh1"], w=["oh2"])
                        P.add("dve", I('tensor_tensor', out=sm[:, 3:4], in0=mx8[:, 1:2], in1=mx8[:, 0:1], op=ALU.subtract), r=["mx8"], w=["sm3"])
                        P.add("act", I('activation', out=sm[:, 3:4], in_=sm[:, 3:4], func=AF.Exp), r=["sm3"], w=["sm3"])
                        P.add("dve", I('tensor_scalar', out=sm[:, 12:13], in0=sm[:, 3:4], scalar1=1.0, scalar2=None, op0=ALU.add), r=["sm3"], w=["sm12"])
                        P.add("dve", I('reciprocal', out=sm[:, 12:13], in_=sm[:, 12:13]), r=["sm12"], w=["sm12"])
                        P.add("dve", I('tensor_tensor', out=wts[:, t, 0:1], in0=sm[:, 12:13], in1=sm[:, 2:3], op=ALU.mult),
                              r=["sm12", "sm2"], w=["wts"])
                        P.add("dve", I('tensor_tensor', out=wts[:, t, 1:2], in0=wts[:, t, 0:1], in1=sm[:, 3:4], op=ALU.mult),
                              r=["wts", "sm3"], w=["wts"])
                        P.add("pe", I('matmul', out=pb(3)[:, 0:32], lhsT=ltri, rhs=sel[:], start=True, stop=True), r=["sel", "cst"], w=["ps3"])
                        P.add("pe", I('matmul', out=pb(3)[:, 32:64], lhsT=ones, rhs=sel[:], start=True, stop=True), r=["sel", "cst"], w=["ps3"])
                        P.add("dve", I('tensor_tensor', out=posm[:], in0=pb(3)[:, 0:32], in1=run_bc[:], op=ALU.add), r=["ps3", "run_bc"], w=["posm"])
                        P.add("dve", I('tensor_tensor', out=run_bc[:], in0=pb(3)[:, 32:64], in1=run_bc[:], op=ALU.add), r=["ps3", "run_bc", "posm"], w=["run_bc"])
                        P.add("dve", I('tensor_scalar', out=ovr[:], in0=posm[:], scalar1=float(CAP), scalar2=BIG, op0=ALU.is_ge, op1=ALU.mult),
                              r=["posm"], w=["ovr"])
                        P.add("dve", I('tensor_tensor', out=posm[:], in0=posm[:], in1=cst[:, C_ECAP:C_ECAP + 32], op=ALU.add), r=["posm", "cst"], w=["posm"])
                        P.add("dve", I('tensor_tensor', out=posm[:], in0=posm[:], in1=ovr[:], op=ALU.add), r=["posm", "ovr"], w=["posm"])
                        P.add("dve", I('tensor_tensor', out=oh1[:], in0=oh1[:], in1=posm[:], op=ALU.mult), r=["oh1", "posm", "oh2"], w=["oh1"])
                        P.add("dve", I('tensor_tensor', out=oh2[:], in0=oh2[:], in1=posm[:], op=ALU.mult), r=["oh2", "posm"], w=["oh2"])
                        P.add("dve", I('reduce_sum', out=sm[:, 13:14], in_=oh1[:], axis=AX.X), r=["oh1"], w=["sm13"])
                        P.add("dve", I('reduce_sum', out=sm[:, 14:15], in_=oh2[:], axis=AX.X), r=["oh2"], w=["sm13"])
                        P.add("dve", I('tensor_copy', out=slots_i[:, 2 * t:2 * t + 2], in_=sm[:, 13:15]), r=["sm13"], w=["slots"])
                        for kk in range(2):
                            P.add("pool", I('indirect_dma_start',
                                out=Xs[:, :], out_offset=bass.IndirectOffsetOnAxis(ap=slots_i[:, 2 * t + kk:2 * t + kk + 1], axis=0),
                                in_=h2b[:, :], in_offset=None, bounds_check='BCREG', oob_is_err=False),
                                r=["h2b", "slots"], w=["Xs_%d_%d" % (t, kk)], dma=True)

                    P.local = {"xt", "junk", "h2f", "h2b", "h2T", "lgt", "msk", "sel", "oh1", "oh2", "posm", "ovr", "mx8",
                               "sm0", "sm1", "sm2", "sm3", "goh", "gexp", "sm12", "sm13", "rstat2", "rstat3"}
                    P.barrier()
                    two_stream(tiles_all, tileM1)

                P.add("dve", I('tensor_copy', out=counts_i[:], in_=run_bc[:]), r=["run_bc"], w=["counts_i"])
                P.barrier()
                if debug:
                    P.add("sp", I('dma_start', out=dbgt[:, 0:68], in_=wts[:].rearrange("p a b -> p (a b)")), r=["wts"], w=["dbg0"], dma=True)
                    P.add("sp", I('dma_start', out=dbgt[:, 68:136], in_=slots_i[:].bitcast(F32)), r=["slots"], w=["dbg1"], dma=True)
                    P.add("sp", I('dma_start', out=dbgt[:, 136:1160], in_=gt2_bc[0][:]), r=["gt2bc0"], w=["dbg2"], dma=True)
                with Scope() as ph2:
                    w1b = [sb("w1b%d" % i, [128, 8, FF], BF16, ph2) for i in range(2)]
                    w3b = [sb("w3b%d" % i, [128, 8, FF], BF16, ph2) for i in range(2)]
                    w2b = [sb("w2b%d" % i, [128, 4, D], BF16, ph2) for i in range(2)]
                    xg = [sb("xg%d" % i, [128, GS // 128, D], BF16, ph2) for i in range(2)]
                    XTs = [sb("XT%d" % i, [128, 8, GS], BF16, ph2) for i in range(2)]
                    sils = [sb("sil%d" % i, [128, GS], F32, ph2) for i in range(2)]
                    actTs = [sb("actT%d" % i, [128, 4, GS], BF16, ph2) for i in range(2)]
                    ysb = [sb("ysb%d" % i, [128, D], F32, ph2) for i in range(2)]
                    it = 0
                    for ex in range(NE):
                        wb_ = ex % 2
                        for exl in ([0, 1] if ex == 0 else ([ex + 1] if ex + 1 < NE else [])):
                            wl_ = exl % 2
                            P.add("pool", I('dma_start', out=w1b[wl_][:, :, :], in_=w1[L, exl].rearrange("(k p) f -> p k f", p=128)),
                                  w=["w1b%d" % wl_], dma=True)
                            P.add("pool", I('dma_start', out=w3b[wl_][:, :, :], in_=w3[L, exl].rearrange("(k p) f -> p k f", p=128)),
                                  w=["w3b%d" % wl_], dma=True)
                            P.add("pool", I('dma_start', out=w2b[wl_][:, :, :], in_=w2[L, exl].rearrange("(k p) f -> p k f", p=128)),
                                  w=["w2b%d" % wl_], dma=True)
                        for grp in range(CAP // GS):
                            P.begin_region(counts_i[0:1, ex:ex + 1], grp * GS, "counts_i")
                            xb_ = it % 2
                            XT = XTs[xb_]
                            actT = actTs[xb_]
                            XK = "XT%d" % xb_
                            AK = "actT%d" % xb_
                            it += 1
                            base = ex * CAP + grp * GS
                            for j in range(GS // 128):
                                P.add("sp", I('dma_start', out=xg[xb_][:, j, :], in_=Xs[base + 128 * j:base + 128 * (j + 1), :]),
                                      w=["xg%d_%d" % (xb_, j)], dma=True)
                            for j in range(GS // 128):
                                bank = j % 2
                                trp = pb(bank).bitcast(BF16)
                                for kc in range(8):
                                    P.add("pe", I('transpose', out=trp[:, 128 * kc:128 * (kc + 1)],
                                                                                                    in_=xg[xb_][:, j, 128 * kc:128 * (kc + 1)], identity=ident_bf[:]),
                                          r=["xg%d_%d" % (xb_, j), "identbf"], w=["ps%d" % bank])
                                P.add("dve", I('tensor_copy', out=XT[:, :, 128 * j:128 * (j + 1)], in_=trp.rearrange("p (a b) -> p a b", a=8)),
                                      r=["ps%d" % bank], w=[XK])
                            for fc in range(4):
                                b1 = 2 + 2 * (fc % 2)
                                for kc in range(8):
                                    P.add("pe", I('matmul', out=pb(b1)[:, 0:GS], lhsT=w1b[wb_][:, kc, 128 * fc:128 * (fc + 1)], rhs=XT[:, kc, :],
                                                                                                start=(kc == 0), stop=(kc == 7)), r=[XK, "w1b%d" % wb_], w=["ps%d" % b1])
                                for kc in range(8):
                                    P.add("pe", I('matmul', out=pb(b1 + 1)[:, 0:GS], lhsT=w3b[wb_][:, kc, 128 * fc:128 * (fc + 1)], rhs=XT[:, kc, :],
                                                                                                start=(kc == 0), stop=(kc == 7)), r=[XK, "w3b%d" % wb_], w=["ps%d" % (b1 + 1)])
                                sil = sils[fc % 2]
                                SK = "sil%d" % (fc % 2)
                                P.add("act", I('activation', out=sil[:], in_=pb(b1)[:, 0:GS], func=AF.Silu), r=["ps%d" % b1], w=[SK])
                                P.add("dve", I('tensor_tensor', out=actT[:, fc, :], in0=pb(b1 + 1)[:, 0:GS], in1=sil[:], op=ALU.mult),
                                      r=["ps%d" % (b1 + 1), SK], w=[AK])
                            for j in range(GS // 128):
                                yb_ = j % 2
                                for nb in range(2):
                                    for fc in range(4):
                                        P.add("pe", I('matmul', out=pb(6 + nb), lhsT=actT[:, fc, 128 * j:128 * (j + 1)],
                                                                                                  rhs=w2b[wb_][:, fc, 512 * nb:512 * (nb + 1)], start=(fc == 0), stop=(fc == 3)),
                                              r=[AK, "w2b%d" % wb_], w=["ps%d" % (6 + nb)])
                                P.add("dve", I('tensor_copy', out=ysb[yb_][:], in_=pb(6, 2)), r=["ps6", "ps7"], w=["ysb%d" % yb_])
                                P.add("sp", I('dma_start', out=Yall[base + 128 * j:base + 128 * (j + 1), :], in_=ysb[yb_][:]),
                                      r=["ysb%d" % yb_], w=["Yall_%d_%d" % (base, j)], dma=True)
                            P.end_region()

                P.barrier()
                with Scope() as ph3:
                    xt = sb2("xt", [128, D], F32, ph3)
                    y0 = sb2("y0", [128, D], F32, ph3)
                    y1 = sb2("y1", [128, D], F32, ph3)
                    junk = sb2("junk", [128, D], BF16, ph3)
                    last = (layer == n_layers - 1)
                    if last:
                        gfin_bc = sb("gfin_bc", [128, D], F32, ph3)
                        P.add("sp", I('dma_start', out=gfin_bc[:], in_=g_fin.partition_broadcast(128)), w=["gfin"], dma=True)
                    def tileM3(t):
                        cond = 0 if t < NT else 1
                        P.add("sp", I('dma_start', out=xt[:], in_=xs_tile(t)), r=["xs%d" % t], w=["xt"], dma=True)
                        P.add("dve", I('memset', y0[:], 0.0), w=["y0"])
                        P.add("dve", I('memset', y1[:], 0.0), w=["y1"])
                        for kk, yy, yk in ((0, y0, "y0"), (1, y1, "y1")):
                            P.add("pool", I('indirect_dma_start',
                                out=yy[:, :], out_offset=None, in_=Yall[:, :],
                                in_offset=bass.IndirectOffsetOnAxis(ap=slots_i[:, 2 * t + kk:2 * t + kk + 1], axis=0),
                                bounds_check='BCREG', oob_is_err=False), r=["slots"], w=[yk], dma=True)
                        if debug and t == 0:
                            P.add("sp", I('dma_start', out=dbgt[:, 1160:2184], in_=y0[:]), r=["y0"], w=["dbg3"], dma=True)
                        P.add("dve", I('tensor_scalar', out=y0[:], in0=y0[:], scalar1=wts[:, t, 0:1], scalar2=None, op0=ALU.mult),
                              r=["y0", "wts"], w=["y0"])
                        P.add("dve", I('scalar_tensor_tensor', out=y0[:], in0=y1[:], scalar=wts[:, t, 1:2], in1=y0[:], op0=ALU.mult, op1=ALU.add),
                              r=["y0", "y1", "wts"], w=["y0"])
                        P.add("dve", I('tensor_tensor', out=y0[:], in0=y0[:], in1=gt2_bc[cond][:], op=ALU.mult),
                              r=["y0", "gt2bc%d" % cond], w=["y0"])
                        P.add("dve", I('tensor_tensor', out=y0[:], in0=y0[:], in1=xt[:], op=ALU.add), r=["y0", "xt"], w=["y0"])
                        if not last:
                            P.add("pool", I('dma_start', out=xs_tile(t), in_=y0[:]), r=["y0"], w=["xs%d" % t], dma=True)
                        elif t < NT:
                            c0, k = rms_rstd(y0[:], "y0", junk[:], "junk", 2 + CUR["st"])
                            P.add("dve", I('scalar_tensor_tensor', out=y1[:], in0=y0[:], scalar=c0, in1=gfin_bc[:], op0=ALU.mult, op1=ALU.mult),
                                  r=["y0", k, "gfin"], w=["y1"])
                            P.add("pool", I('dma_start', out=out_d[128 * t:128 * (t + 1), :], in_=y1[:]), r=["y1"], w=["out%d" % t], dma=True)

                    P.local = {"xt", "y0", "y1", "junk", "rstat2", "rstat3"}
                    extra = None
                    if layer + 1 < n_layers:
                        wst1 = [sb("wadan%d" % i, [128, 8, 512], F32, ph3) for i in range(2)]
                        P.rec = []
                        phase0(layer + 1, wst1)
                        extra = P.rec
                        P.rec = None
                    two_stream(tiles_all, tileM3, extra=extra)

          except _Stop:
            break

        P.stats_peak = peak[0]
        with nc.Block() as block:
            P.emit(nc, block, es)
    return nc, P


_CACHE = {}


def prep_inputs(inputs):
    f = lambda a: np.ascontiguousarray(a, dtype=np.float32)
    c = inputs["c"]
    c_ctx = inputs["c_ctx"]
    shared = {
        "w_ada": f(inputs["w_ada"]),
        "b_adaT": f(inputs["b_ada"].reshape(2, 48, 128).transpose(0, 2, 1)),
        "g_mixT": f(inputs["g_mix"].reshape(2, 8, 128).transpose(0, 2, 1)),
        "g_ffnT": f(inputs["g_ffn"].reshape(2, 8, 128).transpose(0, 2, 1)),
        "g_sguT": f(inputs["g_sgu"].reshape(2, 8, 128).transpose(0, 2, 1)),
        "g_final": f(inputs["g_final"].reshape(1, D)),
        "w_in": f(inputs["w_in"]),
        "w_sT": f(inputs["w_s"].transpose(0, 1, 3, 2)),
        "b_s": f(inputs["b_s"].reshape(2, 1, D)),
        "decay_logit": f(inputs["decay_logit"].reshape(2, 1, 8)),
        "w_pa": f(inputs["w_pa"]),
        "w_pb": f(inputs["w_pb"]),
        "w_out": f(inputs["w_out"]),
        "w_r": f(np.concatenate([inputs["w_group"], inputs["w_erouter"]], axis=2)),
        "b_r": f(np.concatenate([inputs["b_group"], inputs["b_erouter"]], axis=1).reshape(2, 1, 36)),
        "w1": f(inputs["w1"]),
        "w3": f(inputs["w3"]),
        "w2": f(inputs["w2"]),
        "consts": make_consts(),
        "rot": make_rot(),
    }
    in_maps = []
    for b in range(8):
        m = dict(shared)
        m["x"] = f(inputs["x"][b])
        m["ctx"] = f(inputs["ctx"][b])
        cv = np.stack([np.asarray(c[b]).reshape(8, 128).T, np.asarray(c_ctx).reshape(8, 128).T], axis=-1)
        m["cvec"] = f(cv)
        in_maps.append(m)
    return in_maps


def kernel(**inputs):
    inputs = {k: np.asarray(v) for k, v in inputs.items()}
    if "nc" not in _CACHE:
        _CACHE["nc"] = build()[0]
    nc = _CACHE["nc"]
    in_maps = prep_inputs(inputs)
    res = run_bass_kernel_spmd(nc, in_maps, core_ids=list(range(8)))
    out = np.stack([np.asarray(r["out"], dtype=np.float32) for r in res.results], axis=0)
    return out
```

```python
import numpy as np
from contextlib import ExitStack
import concourse.bass as bass
import concourse.mybir as mybir
from concourse.bass_utils import run_bass_kernel_spmd

F32 = mybir.dt.float32
BF16 = mybir.dt.bfloat16
I32 = mybir.dt.int32
AF = mybir.ActivationFunctionType
ALU = mybir.AluOpType
AX = mybir.AxisListType

D = 1024
SEQ = 4096
CTX = 256
NT = SEQ // 128
NCT = CTX // 128
DIN = 7168
NE = 32
FF = 512
CAP = 1536
GS = 256
import os
CUT = int(os.environ.get('KCUT', '99'))
KB = CAP // 128
EPS = 1e-6
QSCALE = 128.0 ** -0.5
NSLOT = 12
BIG = float(NE * CAP)

C_ID = 0
C_ONES = 128
C_LTRI = 256
C_MRF = 384
C_MKF = 512
C_MRB = 640
C_MKB = 768
C_R1 = 896
C_R2 = 1024
C_COL = 1152
C_ECAP = 1156
NCONST = 1188


def make_consts():
    c = np.zeros((128, NCONST), np.float32)
    i = np.arange(128, dtype=np.float32)
    c[:, C_ID:C_ID + 128] = np.eye(128, dtype=np.float32)
    c[:, C_ONES:C_ONES + 128] = 1.0
    c[:, C_LTRI:C_LTRI + 128] = (i[:, None] < i[None, :]).astype(np.float32)
    m = i[:, None]
    n = i[None, :]
    c[:, C_MRF:C_MRF + 128] = np.maximum(n - m, 0)
    c[:, C_MKF:C_MKF + 128] = (n >= m).astype(np.float32) * QSCALE
    c[:, C_MRB:C_MRB + 128] = np.maximum(m - n, 0)
    c[:, C_MKB:C_MKB + 128] = (m > n).astype(np.float32) * QSCALE
    c[:, C_R1:C_R1 + 128] = (i + 1.0)[None, :]
    c[:, C_R2:C_R2 + 128] = (128.0 - i)[None, :]
    c[:, C_COL + 0] = 127.0 - i
    c[:, C_COL + 1] = i
    c[:, C_COL + 2] = 255.0 - i
    c[:, C_COL + 3] = 128.0 + i
    c[:, C_ECAP:C_ECAP + 32] = (np.arange(32, dtype=np.float32) * CAP)[None, :]
    return c


def make_rot():
    nf = 32
    inv = 10000.0 ** (-np.arange(nf, dtype=np.float32) / nf)
    pos = np.arange(SEQ, dtype=np.float32)
    row = np.floor(pos / 64.0)
    col = pos - row * 64.0
    ang = np.concatenate([row[:, None] * inv, col[:, None] * inv], axis=-1).astype(np.float32)
    cos = np.cos(ang).astype(np.float32)
    sin = np.sin(ang).astype(np.float32)
    rot = np.zeros((NT + 1, 128, 256), np.float32)
    cc = np.repeat(cos, 2, axis=1)
    ss = np.stack([-sin, sin], axis=-1).reshape(SEQ, 128)
    rot[:NT, :, 0:128] = cc.reshape(NT, 128, 128)
    rot[:NT, :, 128:256] = ss.reshape(NT, 128, 128)
    rot[NT, :, 0:128] = 1.0
    return rot


ENG_COMPUTE = ("pe", "act", "dve", "pool")
QUEUES = ("sp", "pool", "act")


CUR = {"st": 0, "on": False, "bank": False}


class Dual:
    def __init__(self, a, b):
        self.v = (a, b)

    def __getitem__(self, idx):
        return self.v[CUR["st"]][idx]


def I(name, *args, **kw):
    return (name, args, kw)


class Prog:
    def __init__(self):
        self.ops = []
        self.lastw = {}
        self.lastr = {}
        self.bar_pending = {e: set() for e in ("pe", "act", "dve", "pool", "sp")}
        self.last_on = {}
        self.dmas_open = []
        self.cur_region = None
        self.regions = []
        self.local = set()
        self.rec = None

    def _k(self, k):
        if not CUR["on"]:
            return k
        if k in self.local:
            return k + "@%d" % CUR["st"]
        if CUR["bank"] and k.startswith("ps") and k[2:].isdigit():
            return "ps%d" % (int(k[2:]) + 4 * CUR["st"])
        return k

    def merge(self, lists, lag):
        items = []
        for s_, l in enumerate(lists):
            for i_, op in enumerate(l):
                items.append((i_ + (lag if s_ == 1 else 0) + 0.25 * s_, s_, i_, op))
        items.sort(key=lambda x: (x[0], x[1]))
        for _, _, _, (eng, fn, r, w, dma) in items:
            self._add(eng, fn, r, w, dma)

    def add(self, eng, fn, r=(), w=(), dma=False):
        r = [self._k(k) for k in r]
        w = [self._k(k) for k in w]
        if self.rec is not None:
            self.rec.append((eng, fn, r, w, dma))
            return None
        return self._add(eng, fn, r, w, dma)

    def _add(self, eng, fn, r=(), w=(), dma=False):
        i = len(self.ops)
        deps = set()
        for k in r:
            deps.update(self.lastw.get(k, {}).values())
        for k in w:
            deps.update(self.lastw.get(k, {}).values())
            deps.update(self.lastr.get(k, {}).values())
        deps.update(self.bar_pending[eng])
        self.bar_pending[eng] = set()
        stream = ("d", i) if dma else eng
        for k in r:
            self.lastr.setdefault(k, {})[stream] = i
        for k in w:
            self.lastw[k] = {stream: i}
            self.lastr[k] = {}
        deps.discard(i)
        self.op_region = getattr(self, "op_region", [])
        self.op_region.append(self.cur_region)
        self.ops.append((eng, fn, sorted(deps), dma))
        if dma:
            self.dmas_open.append(i)
        else:
            self.last_on[eng] = i
        return i

    def begin_region(self, cond_ap, thr, cond_key):
        self.regions.append((cond_ap, thr, sorted(self.lastw.get(cond_key, {}).values())))
        self.cur_region = len(self.regions) - 1

    def end_region(self):
        self.cur_region = None

    def barrier(self):
        deps = set(self.last_on.values()) | set(self.dmas_open)
        self.dmas_open = []
        for e in self.bar_pending:
            self.bar_pending[e] = set(deps) | self.bar_pending[e]

    def emit(self, nc, block, es):
        ops = self.ops
        n = len(ops)
        pos_on = [0] * n
        cpos = {e: 0 for e in ENG_COMPUTE}
        for i, (eng, fn, deps, dma) in enumerate(ops):
            if not dma:
                cpos[eng] += 1
                pos_on[i] = cpos[eng]
        fdeps = []
        has_dep = [False] * n
        for (_, _, wr) in self.regions:
            for j in wr:
                has_dep[j] = True
        last_reg = {e: (0, None) for e in ENG_COMPUTE}
        last_other = {e: 0 for e in ENG_COMPUTE}
        for i, (eng, fn, deps, dma) in enumerate(ops):
            ri = self.op_region[i]
            fl = []
            for j in deps:
                je, _, _, jd = ops[j]
                if not jd and je == eng and not dma:
                    if eng == "pe":
                        continue
                    if eng in ("act", "dve") and pos_on[j] < pos_on[i] - 2:
                        m = last_reg[eng][0] if last_reg[eng][1] != ri else last_other[eng]
                        if m <= pos_on[j]:
                            continue
                fl.append(j)
                has_dep[j] = True
            fdeps.append(fl)
            if not dma and ri is not None:
                if last_reg[eng][1] != ri:
                    last_other[eng] = last_reg[eng][0]
                last_reg[eng] = (pos_on[i], ri)
        sems = {e: es.enter_context(nc.semaphore("s_" + e)) for e in ENG_COMPUTE}
        dsem = {q: [es.enter_context(nc.semaphore("d_%s%d" % (q, s))) for s in range(NSLOT)] for q in QUEUES}
        sig = [None] * n
        cnt = {e: 0 for e in ENG_COMPUTE}
        dcnt = {q: 0 for q in QUEUES}
        prevslot = [None] * n
        for i, (eng, fn, deps, dma) in enumerate(ops):
            if dma:
                k = dcnt[eng]
                dcnt[eng] += 1
                s = k % NSLOT
                sig[i] = (("d", eng, s), 16 * (k // NSLOT + 1))
                if k >= NSLOT:
                    prevslot[i] = (("d", eng, s), 16 * (k // NSLOT))
            else:
                if has_dep[i]:
                    cnt[eng] += 1
                    sig[i] = (("c", eng), cnt[eng])

        def semof(key):
            return sems[key[1]] if key[0] == "c" else dsem[key[1]][key[2]]

        per = {e: [] for e in ("pe", "act", "dve", "pool", "sp")}
        waited = {e: {} for e in per}
        cur_reg = {e: None for e in per}
        snap = {e: None for e in per}
        sigcount = {}
        for i, (eng, fn, deps, dma) in enumerate(ops):
            reg = self.op_region[i]
            if reg != cur_reg[eng]:
                if cur_reg[eng] is not None:
                    waited[eng] = snap[eng]
                condw = []
                if reg is not None:
                    for j in self.regions[reg][2]:
                        key, val = sig[j]
                        if waited[eng].get(key, 0) < val:
                            waited[eng][key] = val
                            condw.append((semof(key), val))
                    snap[eng] = dict(waited[eng])
                cur_reg[eng] = reg
            else:
                condw = []
            waits = {}
            for j in fdeps[i]:
                key, val = sig[j]
                if waits.get(key, 0) < val:
                    waits[key] = val
            if prevslot[i] is not None:
                key, val = prevslot[i]
                if waits.get(key, 0) < val:
                    waits[key] = val
            wl = []
            for key, val in waits.items():
                if waited[eng].get(key, 0) >= val:
                    continue
                waited[eng][key] = val
                wl.append((semof(key), val))
            inc = None
            before = None
            if sig[i] is not None:
                inc = (semof(sig[i][0]), 16 if dma else 1)
                before = (sig[i][0], sig[i][1] - (16 if dma else 1))
            per[eng].append((wl, fn, inc, reg, before, condw))
        final = []
        for q in QUEUES:
            for s in range(NSLOT):
                k = dcnt[q]
                uses = (k - s + NSLOT - 1) // NSLOT if k > s else 0
                if uses > 0:
                    final.append((dsem[q][s], 16 * uses))
        for e in ENG_COMPUTE:
            if cnt[e] > 0:
                final.append((sems[e], cnt[e]))
        self.stats = dict(n_ops=n, cnt=cnt, dcnt=dcnt)

        def mk(eng, with_final):
            def f(e):
                bcreg = None
                if eng == "pool":
                    bcreg = e.alloc_register("bcreg")
                    e.reg_mov(bcreg, NE * CAP - 1)
                creg = e.alloc_register("creg_" + eng) if self.regions else None

                def emit_one(wl, fn, inc):
                    if fn[2].get("bounds_check", None) == "BCREG":
                        fn = (fn[0], fn[1], dict(fn[2], bounds_check=bcreg))
                    for s_, v in wl:
                        e.wait_ge(s_, v)
                    try:
                        ins = getattr(e, fn[0])(*fn[1], **fn[2])
                    except Exception:
                        print("EMIT FAIL", eng, fn[0], {k: str(v)[:200] for k, v in fn[2].items()})
                        raise
                    if inc is not None:
                        ins.then_inc(inc[0], inc[1])

                lst = per[eng]
                k = 0
                while k < len(lst):
                    reg = lst[k][3]
                    if reg is None:
                        emit_one(*lst[k][:3])
                        k += 1
                        continue
                    k2 = k
                    while k2 < len(lst) and lst[k2][3] == reg:
                        k2 += 1
                    grp = lst[k:k2]
                    cond_ap, thr, _ = self.regions[reg]
                    for s_, v in grp[0][5]:
                        e.wait_ge(s_, v)
                    e.reg_load(creg, cond_ap)
                    with e.If_cmp(creg, thr, "IS_GT"):
                        for (wl, fn, inc, _, _, _) in grp:
                            emit_one(wl, fn, inc)
                    comp = {}
                    for (wl, fn, inc, _, before, _) in grp:
                        if inc is None:
                            continue
                        key = before[0]
                        if key not in comp:
                            comp[key] = [before[1], 0, inc[0]]
                        comp[key][1] += inc[1]
                    with e.Else():
                        for key, (bval, tot, semh) in comp.items():
                            if bval > 0:
                                e.wait_ge(semh, bval)
                            e.sem_inc(semh, tot)
                    k = k2
                if with_final:
                    for s_, v in final:
                        e.wait_ge(s_, v)
            return f

        block.sync(mk("sp", True))
        block.scalar(mk("act", False))
        block.vector(mk("dve", False))
        block.gpsimd(mk("pool", False))
        block.tensor(mk("pe", False))


class _Stop(Exception):
    pass


def build(n_layers=2, debug=False, stop_after=None):
    nc = bass.Bass("TRN2", target_bir_lowering=False)
    P = Prog()

    def din(name, shape, dt=F32):
        return nc.dram_tensor(name, list(shape), dt, kind="ExternalInput").ap()

    x_in = din("x", [SEQ, D])
    ctx_in = din("ctx", [CTX, D])
    cvec = din("cvec", [128, 8, 2])
    w_ada = din("w_ada", [2, D, 6 * D])
    b_adaT = din("b_adaT", [2, 128, 48])
    g_mixT = din("g_mixT", [2, 128, 8])
    g_ffnT = din("g_ffnT", [2, 128, 8])
    g_sguT = din("g_sguT", [2, 128, 8])
    g_fin = din("g_final", [1, D])
    w_in = din("w_in", [2, D, DIN])
    w_sT = din("w_sT", [2, 8, 128, 128])
    b_s = din("b_s", [2, 1, D])
    dlog = din("decay_logit", [2, 1, 8])
    w_pa = din("w_pa", [2, D, D])
    w_pb = din("w_pb", [2, D, D])
    w_out = din("w_out", [2, D, D])
    w_r = din("w_r", [2, D, 36])
    b_r = din("b_r", [2, 1, 36])
    w1 = din("w1", [2, NE, D, FF])
    w3 = din("w3", [2, NE, D, FF])
    w2 = din("w2", [2, NE, FF, D])
    consts_d = din("consts", [128, NCONST])
    rot_d = din("rot", [NT + 1, 128, 256])
    out_d = nc.dram_tensor("out", [SEQ, D], F32, kind="ExternalOutput").ap()

    def dscr(name, shape, dt=F32):
        kind = "ExternalOutput" if debug else "Internal"
        return nc.dram_tensor(name, list(shape), dt, kind=kind).ap()

    xs = dscr("xs", [SEQ + CTX, D])
    As = dscr("As", [SEQ + CTX, D], BF16)
    Ts = dscr("Ts", [NT + 1, 128, 1024], BF16)
    Xs = dscr("Xs", [NE * CAP, D], BF16)
    Yall = dscr("Yall", [NE * CAP, D])
    Hs = dscr("Hs", [NT + NCT, 128, 1024], BF16)
    dbgt = dscr("dbgt", [128, 68 + 68 + 1024 + 1024]) if debug else None

    es = ExitStack()
    with es:
        ARENA_WORDS = 52000
        arena = es.enter_context(nc.sbuf_tensor("arena", [128, ARENA_WORDS], F32))
        top = [0]
        peak = [0]

        class Scope:
            def __enter__(self):
                self.m = top[0]
                return self

            def __exit__(self, *a):
                top[0] = self.m
                return False

        def sb(name, shape, dt=F32, stack=None):
            nel = 1
            for d_ in shape[1:]:
                nel *= d_
            words = (nel * (4 if dt in (F32, I32) else 2) + 3) // 4
            words = (words + 7) // 8 * 8
            off = top[0]
            top[0] += words
            peak[0] = max(peak[0], top[0])
            assert top[0] <= ARENA_WORDS, ("SBUF arena overflow", name, top[0])
            ap = arena[:, off:off + words]
            if dt != F32:
                ap = ap.bitcast(dt)
            ap = ap[:, 0:nel]
            if len(shape) == 3:
                ap = ap.rearrange("p (a b) -> p a b", a=shape[1])
            return ap

        psum = es.enter_context(nc.psum_tensor("psum", [128, 4096], F32))

        def pb(b, n=1):
            if CUR["bank"] and CUR["st"] == 1:
                b = b + 4
            return psum[:, 512 * b:512 * (b + n)]

        def sb2(name, shape, dt=F32, stack=None):
            return Dual(sb(name + "a", shape, dt), sb(name + "b", shape, dt))

        def two_stream(tiles, tile_fn, lag_frac=0.5, extra=None):
            lists = [[], []]
            ntile_ops = None
            CUR["on"] = True
            CUR["bank"] = True
            for i_, t_ in enumerate(tiles):
                CUR["st"] = i_ % 2
                P.rec = []
                tile_fn(t_)
                if ntile_ops is None:
                    ntile_ops = len(P.rec)
                lists[i_ % 2].extend(P.rec)
            P.rec = None
            CUR["on"] = False
            CUR["bank"] = False
            CUR["st"] = 0
            if extra:
                lists.append(extra)
            P.merge(lists, int((ntile_ops or 0) * lag_frac))

        def pbk(b, n=1):
            return ["ps%d" % (b + i) for i in range(n)]

        cst = sb("cst", [128, NCONST])
        ident_bf = sb("ident_bf", [128, 128], BF16)
        dl_bc = sb("dl_bc", [128, 8])
        lg = sb("lg", [128, 8])
        cdec = sb("cdec", [128, 8])
        DT = sb("DT", [128, 4, 128])
        qdf = sb("qdf", [128, 4, 128])
        qdb = sb("qdb", [128, 4, 128])
        KD = sb("KD", [128, 4, 8])
        tmpA = sb("tmpA", [128, 128])
        tmpB = sb("tmpB", [128, 128])
        scv = sb("scv", [128, 8, 2])
        modT = sb("modT", [128, 48, 2])
        badaT = sb("badaT", [128, 48])
        gmixT = sb("gmixT", [128, 8])
        gffnT = sb("gffnT", [128, 8])
        gsguT = sb("gsguT", [128, 8])
        a1 = sb("a1", [128, 8, 2])
        a2 = sb("a2", [128, 8, 2])
        small = sb("small", [128, 64])
        rstat = sb("rstat", [128, 8])

        ident = cst[:, C_ID:C_ID + 128]
        ones = cst[:, C_ONES:C_ONES + 128]
        ltri = cst[:, C_LTRI:C_LTRI + 128]

        P.add("sp", I('dma_start', out=cst[:], in_=consts_d), w=["cst"], dma=True)
        P.add("sp", I('dma_start', out=scv[:], in_=cvec), w=["scv"], dma=True)
        P.add("dve", I('tensor_copy', out=ident_bf[:], in_=ident), r=["cst"], w=["identbf"])
        P.add("act", I('activation', out=scv[:], in_=scv[:], func=AF.Silu), r=["scv"], w=["scv"])

        def src_tile(layer, t):
            if layer == 0:
                return x_in[128 * t:128 * (t + 1), :] if t < NT else ctx_in[128 * (t - NT):128 * (t - NT + 1), :]
            return xs[128 * t:128 * (t + 1), :]

        def xs_tile(t):
            return xs[128 * t:128 * (t + 1), :]

        def rms_rstd(xt, xkey, junk, junkkey, col):
            c0 = rstat[:, col:col + 1]
            k = "rstat%d" % col
            P.add("act", I('activation', out=junk, in_=xt, func=AF.Square, accum_out=c0),
                  r=[xkey], w=[junkkey, k])
            P.add("dve", I('tensor_scalar', out=c0, in0=c0, scalar1=1.0 / D, scalar2=EPS,
                                                   op0=ALU.mult, op1=ALU.add), r=[k], w=[k])
            P.add("act", I('activation', out=c0, in_=c0, func=AF.Sqrt), r=[k], w=[k])
            P.add("dve", I('reciprocal', out=c0, in_=c0), r=[k], w=[k])
            return c0, k

        for layer in range(n_layers):
          try:
            L = layer
            has_ctx = (layer == 0)
            tiles_all = list(range(NT)) + ([NT, NT + 1] if has_ctx else [])

            def phase0(L, wst):
                P.add("sp", I('dma_start', out=badaT[:], in_=b_adaT[L]), w=["badaT"], dma=True)
                P.add("sp", I('dma_start', out=gmixT[:], in_=g_mixT[L]), w=["gmixT"], dma=True)
                P.add("sp", I('dma_start', out=gffnT[:], in_=g_ffnT[L]), w=["gffnT"], dma=True)
                P.add("sp", I('dma_start', out=gsguT[:], in_=g_sguT[L]), w=["gsguT"], dma=True)
                P.add("sp", I('dma_start', out=dl_bc[:], in_=dlog[L].partition_broadcast(128)), w=["dl"], dma=True)
                for blk in range(12):
                    wt = wst[blk % 2]
                    wk = "wada%d" % (blk % 2)
                    for kc in range(8):
                        P.add("sp", I('dma_start',
                            out=wt[:, kc, :], in_=w_ada[L, 128 * kc:128 * (kc + 1), 512 * blk:512 * (blk + 1)]),
                            w=[wk + "_%d" % kc], dma=True)
                    for jj in range(4):
                        j = blk * 4 + jj
                        for kc in range(8):
                            P.add("pe", I('matmul',
                                out=pb(0)[:, 2 * j:2 * j + 2], lhsT=wt[:, kc, 128 * jj:128 * (jj + 1)],
                                rhs=scv[:, kc, :], start=(kc == 0), stop=(kc == 7)),
                                r=[wk + "_%d" % kc, "scv"], w=["ps0"])
                mod2 = modT[:].rearrange("p j t -> p (j t)")
                P.add("dve", I('tensor_copy', out=mod2, in_=pb(0)[:, 0:96]), r=["ps0"], w=["modT"])
                for t in range(2):
                    P.add("dve", I('tensor_tensor', out=modT[:, :, t], in0=modT[:, :, t], in1=badaT[:],
                                                                op=ALU.add), r=["modT", "badaT"], w=["modT"])
                for t in range(2):
                    P.add("dve", I('scalar_tensor_tensor',
                        out=a1[:, :, t], in0=modT[:, 8:16, t], scalar=1.0, in1=gmixT[:], op0=ALU.add, op1=ALU.mult),
                        r=["modT", "gmixT"], w=["a1"])
                    P.add("dve", I('scalar_tensor_tensor',
                        out=a2[:, :, t], in0=modT[:, 32:40, t], scalar=1.0, in1=gffnT[:], op0=ALU.add, op1=ALU.mult),
                        r=["modT", "gffnT"], w=["a2"])
                P.add("act", I('activation', out=lg[:], in_=dl_bc[:], func=AF.Exp, scale=-1.0), r=["dl"], w=["lg"])
                P.add("act", I('activation', out=lg[:], in_=lg[:], func=AF.Ln, bias=1.0), r=["lg"], w=["lg"])
                P.add("dve", I('tensor_scalar', out=lg[:], in0=lg[:], scalar1=-1.0, scalar2=None, op0=ALU.mult),
                      r=["lg"], w=["lg"])
                P.add("act", I('activation', out=cdec[:], in_=lg[:], func=AF.Exp, scale=128.0), r=["lg"], w=["cdec"])
                for h in range(4):
                    lf = lg[:, h:h + 1]
                    lb = lg[:, 4 + h:5 + h]
                    P.add("act", I('activation', out=tmpA[:], in_=cst[:, C_MRF:C_MRF + 128], func=AF.Exp, scale=lf),
                          r=["lg", "cst"], w=["tmpA"])
                    P.add("dve", I('tensor_tensor', out=tmpA[:], in0=tmpA[:], in1=cst[:, C_MKF:C_MKF + 128], op=ALU.mult),
                          r=["tmpA", "cst"], w=["tmpA"])
                    P.add("act", I('activation', out=tmpB[:], in_=cst[:, C_MRB:C_MRB + 128], func=AF.Exp, scale=lb),
                          r=["lg", "cst"], w=["tmpB"])
                    P.add("dve", I('tensor_tensor', out=tmpB[:], in0=tmpB[:], in1=cst[:, C_MKB:C_MKB + 128], op=ALU.mult),
                          r=["tmpB", "cst"], w=["tmpB"])
                    P.add("dve", I('tensor_tensor', out=DT[:, h, :], in0=tmpA[:], in1=tmpB[:], op=ALU.add),
                          r=["tmpA", "tmpB"], w=["DT"])
                    P.add("act", I('activation', out=qdf[:, h, :], in_=cst[:, C_R1:C_R1 + 128], func=AF.Exp, scale=lf),
                          r=["lg", "cst"], w=["qdf"])
                    P.add("act", I('activation', out=qdb[:, h, :], in_=cst[:, C_R2:C_R2 + 128], func=AF.Exp, scale=lb),
                          r=["lg", "cst"], w=["qdb"])
                    for cs in range(4):
                        P.add("act", I('activation',
                            out=KD[:, cs, h:h + 1], in_=cst[:, C_COL + cs:C_COL + cs + 1], func=AF.Exp, scale=lf),
                            r=["lg", "cst"], w=["KD"])
                        P.add("act", I('activation',
                            out=KD[:, cs, 4 + h:5 + h], in_=cst[:, C_COL + cs:C_COL + cs + 1], func=AF.Exp, scale=lb),
                            r=["lg", "cst"], w=["KD"])
                kd2 = KD[:].rearrange("p a b -> p (a b)")
                P.add("dve", I('tensor_scalar', out=kd2, in0=kd2, scalar1=QSCALE, scalar2=None, op0=ALU.mult),
                      r=["KD"], w=["KD"])


            if layer == 0:
                P.barrier()
                with Scope() as ph:
                    wst0 = [sb("wada%d" % i, [128, 8, 512], F32, ph) for i in range(2)]
                    phase0(0, wst0)

            def bcast_gate(dst, dkey, which, t):
                for kc in range(8):
                    col = modT[:, which * 8 + kc, t:t + 1]
                    P.add("dve", I('tensor_scalar', out=tmpA[:], in0=ident, scalar1=col, scalar2=None,
                                                                    op0=ALU.mult), r=["modT", "cst"], w=["tmpA"])
                    P.add("pe", I('matmul', out=pb(kc // 4)[:, 128 * (kc % 4):128 * (kc % 4 + 1)],
                                                          lhsT=ones, rhs=tmpA[:], start=True, stop=True),
                          r=["tmpA", "cst"], w=["ps%d" % (kc // 4)])
                P.add("act", I('activation', out=dst, in_=pb(0, 2), func=AF.Identity), r=["ps0", "ps1"], w=[dkey])

            def bcast_feat(dst, dkey, src, skey, t):
                for kc in range(8):
                    col = src[:, kc, t:t + 1]
                    P.add("dve", I('tensor_scalar', out=tmpA[:], in0=ident, scalar1=col, scalar2=None,
                                                                    op0=ALU.mult), r=[skey, "cst"], w=["tmpA"])
                    P.add("pe", I('matmul', out=pb(kc // 4)[:, 128 * (kc % 4):128 * (kc % 4 + 1)],
                                                          lhsT=ones, rhs=tmpA[:], start=True, stop=True),
                          r=["tmpA", "cst"], w=["ps%d" % (kc // 4)])
                P.add("act", I('activation', out=dst, in_=pb(0, 2), func=AF.Identity), r=["ps0", "ps1"], w=[dkey])

            def load_w_cast(dst3, dkey, src2d, ncols):
                P.add("pool", I('dma_start', out=dst3[:, :, :], in_=src2d.rearrange("(k p) f -> p k f", p=128)),
                      w=[dkey], dma=True)

            def h_tile(ph_bufs, t, layer_src, a_ap, b_ap, cond):
                xt, xnb, hT, junk = ph_bufs
                P.add("sp", I('dma_start', out=xt[:], in_=layer_src), w=["xt"], dma=True)
                c0, k = rms_rstd(xt[:], "xt", junk[:], "junk", 2 + CUR["st"] if CUR["on"] else 0)
                P.add("dve", I('tensor_scalar', out=xnb[:], in0=xt[:], scalar1=c0, scalar2=None, op0=ALU.mult),
                      r=["xt", k], w=["xnb"])
                trp = pb(0).bitcast(BF16)
                for kc in range(8):
                    P.add("pe", I('transpose', out=trp[:, 128 * kc:128 * (kc + 1)],
                                                             in_=xnb[:, 128 * kc:128 * (kc + 1)], identity=ident_bf[:]),
                          r=["xnb", "identbf"], w=["ps0"])
                for kc in range(8):
                    P.add("act", I('activation', out=hT[:, kc, :], in_=trp[:, 128 * kc:128 * (kc + 1)],
                                                               func=AF.Identity, scale=a_ap[:, kc, cond:cond + 1],
                                                               bias=b_ap[:, kc, cond:cond + 1]),
                          r=["ps0", "a1", "a2", "modT"], w=["hT"])

            def proj_tok(hT, wt, wkey, col0, ncol, bank):
                for nb in range(ncol // 512):
                    for kc in range(8):
                        P.add("pe", I('matmul',
                            out=pb(bank + nb), lhsT=hT[:, kc, :], rhs=wt[:, kc, col0 + 512 * nb:col0 + 512 * (nb + 1)],
                            start=(kc == 0), stop=(kc == 7)), r=["hT", wkey], w=["ps%d" % (bank + nb)])

            P.barrier()
            with Scope() as ph:
                wA = sb("wA", [128, 8, 3072], BF16, ph)
                wpa = sb("wpa", [128, 8, 1024], BF16, ph)
                wsT = sb("wsT", [128, 8, 128], BF16, ph)
                bs_bc = sb("bs_bc", [128, 8, 128], F32, ph)
                xt = sb2("xt", [128, D], F32, ph)
                xnb = sb2("xnb", [128, D], BF16, ph)
                hT = sb2("hT", [128, 8, 128], BF16, ph)
                junk = sb2("junk", [128, D], BF16, ph)
                guT = sb2("guT", [128, 8, 128], BF16, ph)
                gv = sb2("gv", [128, D], F32, ph)
                vhat = sb2("vhat", [128, D], BF16, ph)
                bst = sb2("bst", [128, 2, 6], F32, ph)
                mv = sb2("mv", [128, 2], F32, ph)
                yaT = sb2("yaT", [128, 8, 128], BF16, ph)
                sga = sb2("sga", [128, D], F32, ph)
                ao = sb2("ao", [128, D], BF16, ph)
                P.local = {"xt", "xnb", "hT", "junk", "guT", "gv", "vhat", "bst", "mv", "yaT", "sga", "ao",
                           "rstat2", "rstat3"}
                load_w_cast(wA[:, :, 0:2048], "wA", w_in[L][:, 0:2048], 2048)
                load_w_cast(wA[:, :, 2048:3072], "wA", w_in[L][:, 5120:6144], 1024)
                load_w_cast(wpa, "wpa", w_pa[L], 1024)
                for g in range(8):
                    P.add("pool", I('dma_start', out=wsT[:, g, :], in_=w_sT[L, g]), w=["wsT"], dma=True)
                P.add("sp", I('dma_start', out=bs_bc[:].rearrange("p g q -> p (g q)"), in_=b_s[L].partition_broadcast(128)),
                      w=["bs_bc"], dma=True)
                def tileA(t):
                    cond = 0 if t < NT else 1
                    h_tile((xt, xnb, hT, junk), t, src_tile(layer, t), a1, modT[:, 0:8, :], cond)
                    P.add("pool", I('dma_start', out=Hs[t], in_=hT[:].rearrange("p a b -> p (a b)")), r=["hT"], w=["Hs%d" % t], dma=True)
                    if t >= NT and not has_ctx:
                        return
                    for fc in range(8):
                        for kc in range(8):
                            P.add("pe", I('matmul',
                                out=pb(fc // 4)[:, 128 * (fc % 4):128 * (fc % 4 + 1)],
                                lhsT=wA[:, kc, 128 * fc:128 * (fc + 1)], rhs=hT[:, kc, :],
                                start=(kc == 0), stop=(kc == 7)), r=["hT", "wA"], w=["ps%d" % (fc // 4)])
                    P.add("act", I('activation', out=guT[:].rearrange("p a b -> p (a b)"), in_=pb(0, 2), func=AF.Gelu_apprx_tanh),
                          r=["ps0", "ps1"], w=["guT"])
                    proj_tok(hT, wA, "wA", 1024, 1024, 2)
                    P.add("act", I('activation', out=gv[:], in_=pb(2, 2), func=AF.Gelu_apprx_tanh),
                          r=["ps2", "ps3"], w=["gv"])
                    for hh in range(2):
                        P.add("dve", I('bn_stats', out=bst[:, hh, :], in_=gv[:, 512 * hh:512 * (hh + 1)]),
                              r=["gv"], w=["bst"])
                    P.add("dve", I('bn_aggr', out=mv[:], in_=bst[:].rearrange("p a b -> p (a b)")), r=["bst"], w=["mv"])
                    P.add("dve", I('tensor_scalar', out=mv[:, 1:2], in0=mv[:, 1:2], scalar1=EPS, scalar2=None, op0=ALU.add),
                          r=["mv"], w=["mv"])
                    P.add("act", I('activation', out=mv[:, 1:2], in_=mv[:, 1:2], func=AF.Sqrt), r=["mv"], w=["mv"])
                    P.add("dve", I('reciprocal', out=mv[:, 1:2], in_=mv[:, 1:2]), r=["mv"], w=["mv"])
                    P.add("dve", I('tensor_scalar', out=vhat[:], in0=gv[:], scalar1=mv[:, 0:1], scalar2=mv[:, 1:2],
                                                           op0=ALU.subtract, op1=ALU.mult), r=["gv", "mv"], w=["vhat"])
                    for g in range(8):
                        P.add("pe", I('matmul', out=pb(g // 4)[:, 128 * (g % 4):128 * (g % 4 + 1)],
                                                            lhsT=vhat[:, 128 * g:128 * (g + 1)], rhs=wsT[:, g, :],
                                                            start=True, stop=True), r=["vhat", "wsT"], w=["ps%d" % (g // 4)])
                    for g in range(8):
                        P.add("dve", I('scalar_tensor_tensor',
                            out=gv[:, 128 * g:128 * (g + 1)], in0=pb(g // 4)[:, 128 * (g % 4):128 * (g % 4 + 1)],
                            scalar=gsguT[:, g:g + 1], in1=bs_bc[:, g, :], op0=ALU.mult, op1=ALU.add),
                            r=["ps%d" % (g // 4), "gsguT", "bs_bc", "vhat"], w=["gv"])
                    P.add("dve", I('tensor_tensor', out=yaT[:].rearrange("p a b -> p (a b)"), in0=gv[:],
                                                           in1=guT[:].rearrange("p a b -> p (a b)"), op=ALU.mult),
                          r=["gv", "guT"], w=["yaT"])
                    for nb in range(2):
                        for kc in range(8):
                            P.add("pe", I('matmul',
                                out=pb(2 + nb), lhsT=yaT[:, kc, :], rhs=wpa[:, kc, 512 * nb:512 * (nb + 1)],
                                start=(kc == 0), stop=(kc == 7)), r=["yaT", "wpa"], w=["ps%d" % (2 + nb)])
                    proj_tok(hT, wA, "wA", 2048, 1024, 0)
                    P.add("act", I('activation', out=sga[:], in_=pb(0, 2), func=AF.Sigmoid), r=["ps0", "ps1"], w=["sga"])
                    P.add("dve", I('tensor_tensor', out=ao[:], in0=pb(2, 2), in1=sga[:], op=ALU.mult),
                          r=["ps2", "ps3", "sga"], w=["ao"])
                    P.add("pool", I('dma_start', out=As[128 * t:128 * (t + 1), :], in_=ao[:]),
                          r=["ao"], w=["As%d" % t], dma=True)

                two_stream(tiles_all, tileA)
                if not has_ctx:
                    P.barrier()
                    for t_ in (NT, NT + 1):
                        tileA(t_)

            if stop_after == ('A', layer):
                raise _Stop
            P.barrier()
            with Scope() as ph:
                wB = sb("wB", [128, 8, 4096], BF16, ph)
                wpb = sb("wpb", [128, 8, 1024], BF16, ph)
                wout = sb("wout", [128, 8, 1024], BF16, ph)
                gt1_bc = [sb("gt1bc%d" % i, [128, D], F32, ph) for i in range(2 if has_ctx else 1)]
                xt = sb2("xt", [128, D], F32, ph)
                hT = sb2("hT", [128, 8, 128], BF16, ph)
                rt = sb2("rt", [128, 256], F32, ph)
                t1 = sb("t1", [128, D], F32, ph)
                t2 = sb("t2", [128, D], F32, ph)
                t1h = Dual(t1[:, 0:512], t1[:, 512:1024])
                t1d = Dual(t1, sb("t1x", [128, D], F32, ph))
                t2h = Dual(t2[:, 0:512], t2[:, 512:1024])
                qkr = sb2("qkr", [128, 8, 128], BF16, ph)
                qT = sb2("qT", [128, 4, 128], BF16, ph)
                qTf = sb2("qTf", [128, 4, 128], BF16, ph)
                qTb = sb2("qTb", [128, 4, 128], BF16, ph)
                kT = sb2("kT", [128, 4, 128], BF16, ph)
                kd = sb2("kd", [128, 4, 128], BF16, ph)
                vb = sb2("vb", [128, 4, 256], BF16, ph)
                sgr = sb2("sgr", [128, D], BF16, ph)
                sgb = sb2("sgb", [128, D], BF16, ph)
                PT = sb2("PT", [128, 4, 128], BF16, ph)
                yb = sb2("yb", [128, D], BF16, ph)
                tT = sb2("tT", [128, 8, 128], BF16, ph)
                apt = sb2("apt", [128, D], BF16, ph)
                mg = sb2("mg", [128, D], BF16, ph)
                Sfb = sb("Sfb", [128, 4, 256], BF16, ph)
                Tb = sb("Tb", [128, 4, 256], F32, ph)
                Sf = Tb
                Tbb = sb2("Tbb", [128, 4, 256], BF16, ph)
                scf = sb("scf", [128, 4, 256], F32, ph)
                lnst = sb2("lnst", [128, 4, 6], F32, ph)
                lnmv = sb2("lnmv", [128, 4, 2], F32, ph)
                lnb = sb("lnb", [128, 4], F32, ph)

                load_w_cast(wB[:, :, 0:3072], "wB", w_in[L][:, 2048:5120], 3072)
                load_w_cast(wB[:, :, 3072:4096], "wB", w_in[L][:, 6144:7168], 1024)
                load_w_cast(wpb, "wpb", w_pb[L], 1024)
                load_w_cast(wout, "wout", w_out[L], 1024)
                for i in range(len(gt1_bc)):
                    bcast_gate(gt1_bc[i][:], "gt1bc%d" % i, 2, i)

                def state_update(S, skey, kdcol_set, coff, vkey="vb", kvb=6):
                    for h in range(4):
                        P.add("dve", I('tensor_scalar', out=kd[:, h, :], in0=qkr[:, 4 + h, :],
                                                                    scalar1=KD[:, kdcol_set, coff + h:coff + h + 1], scalar2=None,
                                                                    op0=ALU.mult), r=["qkr", "KD"], w=["kd"])
                    for h in range(4):
                        P.add("pe", I('matmul', out=pb(kvb + h // 2)[:, 256 * (h % 2):256 * (h % 2 + 1)],
                                                            lhsT=kd[:, h, :], rhs=vb[:, h, :], start=True, stop=True),
                              r=["kd", vkey], w=["ps%d" % (kvb + h // 2)])
                    if S is not None:
                        for h in range(4):
                            P.add("dve", I('scalar_tensor_tensor',
                                out=S[:, h, :], in0=S[:, h, :], scalar=cdec[:, coff + h:coff + h + 1],
                                in1=pb(kvb + h // 2)[:, 256 * (h % 2):256 * (h % 2 + 1)], op0=ALU.mult, op1=ALU.add),
                                r=[skey, "cdec", "ps%d" % (kvb + h // 2)], w=[skey])

                def rotary(ps_ap, nh, h0, rot_idx, pskeys=("ps1",), t1=t1, t2=t2):
                    pskeys = list(pskeys)
                    P.add("sp", I('dma_start', out=rt[:], in_=rot_d[rot_idx]), w=["rt"], dma=True)
                    w = nh * 128
                    src3 = ps_ap.rearrange("p (h j two) -> p h j two", h=nh, two=2)
                    t2v = t2[:, 0:w].rearrange("p (h j two) -> p h j two", h=nh, two=2)
                    Sv = rt[:, 128:256].rearrange("p (j two) -> p j two", two=2)
                    for hh in range(nh):
                        P.add("dve", I('tensor_tensor', out=t1[:, 128 * hh:128 * (hh + 1)],
                                                                      in0=ps_ap[:, 128 * hh:128 * (hh + 1)], in1=rt[:, 0:128], op=ALU.mult),
                              r=pskeys + ["rt"], w=["t1"])
                        P.add("dve", I('tensor_tensor', out=t2v[:, hh, :, 0], in0=src3[:, hh, :, 1], in1=Sv[:, :, 0], op=ALU.mult),
                              r=pskeys + ["rt"], w=["t2"])
                        P.add("dve", I('tensor_tensor', out=t2v[:, hh, :, 1], in0=src3[:, hh, :, 0], in1=Sv[:, :, 1], op=ALU.mult),
                              r=pskeys + ["rt"], w=["t2"])
                    P.add("dve", I('tensor_tensor', out=qkr[:, h0:h0 + nh, :].rearrange("p a b -> p (a b)"),
                                                            in0=t1[:, 0:w], in1=t2[:, 0:w], op=ALU.add),
                          r=["t1", "t2"], w=["qkr"])

                def load_hT(t):
                    P.add("sp", I('dma_start', out=hT[:].rearrange("p a b -> p (a b)"), in_=Hs[t]), r=["Hs%d" % t], w=["hT"], dma=True)

                def kv_tile(t, cond, rot_idx, kb=1, vbk=2):
                    load_hT(t)
                    proj_tok(hT, wB, "wB", 512, 512, kb)
                    proj_tok(hT, wB, "wB", 1024, 1024, vbk)
                    P.add("act", I('activation', out=vb[:].rearrange("p a b -> p (a b)"), in_=pb(vbk, 2), func=AF.Identity),
                          r=["ps%d" % vbk, "ps%d" % (vbk + 1)], w=["vb"])
                    rotary(pb(kb), 4, 4, rot_idx, ("ps%d" % kb,), t1=t1h, t2=t2h)

                def store_state_bf(S, skey, dst_idx):
                    P.add("act", I('activation', out=Tbb[:].rearrange("p a b -> p (a b)"), in_=S[:].rearrange("p a b -> p (a b)"),
                                                        func=AF.Identity), r=[skey], w=["Tbb"])
                    P.add("pool", I('dma_start', out=Ts[dst_idx], in_=Tbb[:].rearrange("p a b -> p (a b)")),
                          r=["Tbb"], w=["Ts%d" % dst_idx], dma=True)

                for j in range(NCT):
                    kv_tile(NT + j, 1, NT)
                    fset = 2 if j == 0 else 0
                    bset = 1 if j == 0 else 3
                    state_update(None, None, fset, 0)
                    if j == 0:
                        P.add("act", I('activation', out=scf[:].rearrange("p a b -> p (a b)"), in_=pb(6, 2), func=AF.Identity),
                              r=["ps6", "ps7"], w=["scf"])
                    else:
                        P.add("dve", I('tensor_tensor', out=scf[:].rearrange("p a b -> p (a b)"), in0=scf[:].rearrange("p a b -> p (a b)"),
                                                               in1=pb(6, 2), op=ALU.add), r=["scf", "ps6", "ps7"], w=["scf"])
                    state_update(None, None, bset, 4)
                    if j == 0:
                        P.add("act", I('activation', out=Tb[:].rearrange("p a b -> p (a b)"), in_=pb(6, 2), func=AF.Identity),
                              r=["ps6", "ps7"], w=["Tb"])
                    else:
                        P.add("dve", I('tensor_tensor', out=Tb[:].rearrange("p a b -> p (a b)"), in0=Tb[:].rearrange("p a b -> p (a b)"),
                                                               in1=pb(6, 2), op=ALU.add), r=["Tb", "ps6", "ps7"], w=["Tb"])
                        if has_ctx:
                            state_update(None, None, 1, 4)
                            P.add("act", I('activation', out=Tbb[:].rearrange("p a b -> p (a b)"), in_=pb(6, 2), func=AF.Identity),
                                  r=["ps6", "ps7"], w=["Tbb"])
                            P.add("pool", I('dma_start', out=Ts[NT], in_=Tbb[:].rearrange("p a b -> p (a b)")),
                                  r=["Tbb"], w=["Ts%d" % NT], dma=True)
                store_state_bf(Tb, "Tb", NT - 1)
                def b1_tile(c):
                    kv_tile(c, 0, c, kb=0, vbk=1)
                    state_update(Tb, "Tb", 1, 4, kvb=2)
                    store_state_bf(Tb, "Tb", c - 1)

                P.local = {"hT", "rt", "t1", "t2", "qkr", "kd", "vb", "Tbb"}
                P.barrier()
                two_stream(list(range(NT - 1, 0, -1)), b1_tile)
                P.barrier()

                if stop_after == ('B1', layer):
                    raise _Stop
                def fwd_tile(t, cond, rot_idx, first, zero_b):
                    gbc = gt1_bc[cond]
                    gkey = "gt1bc%d" % cond
                    P.add("sp", I('dma_start', out=xt[:], in_=src_tile(layer, t)), w=["xt"], dma=True)
                    load_hT(t)
                    if not zero_b:
                        P.add("sp", I('dma_start', out=Tbb[:].rearrange("p a b -> p (a b)"), in_=Ts[t if t < NT else NT]),
                              r=["Ts%d" % (t if t < NT else NT)], w=["Tbb"], dma=True)
                    P.add("sp", I('dma_start', out=apt[:], in_=As[128 * t:128 * (t + 1), :]), r=["As%d" % t], w=["apt"], dma=True)
                    proj_tok(hT, wB, "wB", 0, 512, 0)
                    proj_tok(hT, wB, "wB", 512, 512, 1)
                    proj_tok(hT, wB, "wB", 1024, 1024, 2)
                    P.add("act", I('activation', out=vb[:].rearrange("p a b -> p (a b)"), in_=pb(2, 2), func=AF.Identity),
                          r=["ps2", "ps3"], w=["vb"])
                    rotary(pb(0), 4, 0, rot_idx, ("ps0",), t1=t1d, t2=t2h)
                    rotary(pb(1), 4, 4, rot_idx, ("ps1",), t1=t1d, t2=t2h)
                    trp = pb(2).bitcast(BF16)
                    trpB = pb(0).bitcast(BF16)
                    trpC = pb(1).bitcast(BF16)
                    for i8 in range(8):
                        P.add("pe", I('transpose', out=trp[:, 128 * i8:128 * (i8 + 1)], in_=qkr[:, i8, :], identity=ident_bf[:]),
                              r=["qkr", "identbf"], w=["ps2"])
                    P.add("act", I('activation', out=qT[:].rearrange("p a b -> p (a b)"), in_=trp[:, 0:512], func=AF.Identity),
                          r=["ps2"], w=["qT"])
                    P.add("act", I('activation', out=kT[:].rearrange("p a b -> p (a b)"), in_=trp[:, 512:1024], func=AF.Identity),
                          r=["ps2"], w=["kT"])
                    P.add("dve", I('tensor_tensor', out=qTf[:].rearrange("p a b -> p (a b)"), in0=qT[:].rearrange("p a b -> p (a b)"),
                                   in1=qdf[:].rearrange("p a b -> p (a b)"), op=ALU.mult), r=["qT", "qdf"], w=["qTf"])
                    P.add("dve", I('tensor_tensor', out=qTb[:].rearrange("p a b -> p (a b)"), in0=qT[:].rearrange("p a b -> p (a b)"),
                                   in1=qdb[:].rearrange("p a b -> p (a b)"), op=ALU.mult), r=["qT", "qdb"], w=["qTb"])
                    for h in range(4):
                        P.add("pe", I('matmul', out=pb(3)[:, 128 * h:128 * (h + 1)], lhsT=kT[:, h, :], rhs=qT[:, h, :],
                                                            start=True, stop=True), r=["kT", "qT"], w=["ps3"])
                    P.add("dve", I('tensor_tensor', out=PT[:].rearrange("p a b -> p (a b)"), in0=pb(3),
                                                           in1=DT[:].rearrange("p a b -> p (a b)"), op=ALU.mult), r=["ps3", "DT"], w=["PT"])
                    if CUT <= 1:
                        return
                    proj_tok(hT, wB, "wB", 2048, 1024, 0)
                    P.add("act", I('activation', out=sgr[:], in_=pb(0, 2), func=AF.Silu), r=["ps0", "ps1"], w=["sgr"])
                    for h in range(4):
                        oap = pb(2 + h // 2)[:, 256 * (h % 2):256 * (h % 2 + 1)]
                        okey = "ps%d" % (2 + h // 2)
                        nterm = 1 + (0 if first else 1) + (0 if zero_b else 1)
                        P.add("pe", I('matmul', out=oap, lhsT=PT[:, h, :], rhs=vb[:, h, :], start=True, stop=(nterm == 1)),
                              r=["PT", "vb"], w=[okey])
                        done = 1
                        if not first:
                            done += 1
                            P.add("pe", I('matmul', out=oap, lhsT=qTf[:, h, :], rhs=Sfb[:, h, :], start=False, stop=(done == nterm)),
                                  r=["qTf", "Sfb"], w=[okey])
                        if not zero_b:
                            done += 1
                            P.add("pe", I('matmul', out=oap, lhsT=qTb[:, h, :], rhs=Tbb[:, h, :], start=False, stop=True),
                                  r=["qTb", "Tbb"], w=[okey])
                    if CUT <= 2:
                        return
                    P.add("act", I('activation', out=t1d[:], in_=pb(2, 2), func=AF.Identity), r=["ps2", "ps3"], w=["t1"])
                    for h in range(4):
                        P.add("dve", I('bn_stats', out=lnst[:, h, :], in_=t1d[:, 256 * h:256 * (h + 1)]), r=["t1"], w=["lnst"])
                    for h in range(4):
                        P.add("dve", I('bn_aggr', out=lnmv[:, h, :], in_=lnst[:, h, :]), r=["lnst"], w=["lnmv"])
                    P.add("dve", I('tensor_scalar', out=lnmv[:, :, 1], in0=lnmv[:, :, 1], scalar1=EPS, scalar2=None, op0=ALU.add),
                          r=["lnmv"], w=["lnmv"])
                    P.add("act", I('activation', out=lnmv[:, :, 1], in_=lnmv[:, :, 1], func=AF.Sqrt), r=["lnmv"], w=["lnmv"])
                    P.add("dve", I('reciprocal', out=lnmv[:, :, 1], in_=lnmv[:, :, 1]), r=["lnmv"], w=["lnmv"])
                    for h in range(4):
                        P.add("dve", I('tensor_scalar', out=t1d[:, 256 * h:256 * (h + 1)], in0=t1d[:, 256 * h:256 * (h + 1)], scalar1=lnmv[:, h, 0:1],
                                       scalar2=lnmv[:, h, 1:2], op0=ALU.subtract, op1=ALU.mult),
                              r=["t1", "lnmv"], w=["t1"])
                    P.add("dve", I('tensor_tensor', out=yb[:], in0=t1d[:], in1=sgr[:], op=ALU.mult), r=["t1", "sgr"], w=["yb"])
                    if CUT <= 3:
                        return
                    state_update(Sf, "Sf", 0, 0, kvb=0)
                    P.add("act", I('activation', out=Sfb[:].rearrange("p a b -> p (a b)"), in_=Sf[:].rearrange("p a b -> p (a b)"),
                                                        func=AF.Identity), r=["Sf"], w=["Sfb"])
                    if CUT <= 4:
                        return
                    proj_tok(hT, wB, "wB", 3072, 1024, 2)
                    P.add("act", I('activation', out=sgb[:], in_=pb(2, 2), func=AF.Sigmoid), r=["ps2", "ps3"], w=["sgb"])
                    for kc in range(8):
                        P.add("pe", I('transpose', out=trpB[:, 128 * kc:128 * (kc + 1)], in_=yb[:, 128 * kc:128 * (kc + 1)], identity=ident_bf[:]),
                              r=["yb", "identbf"], w=["ps0"])
                    P.add("act", I('activation', out=tT[:].rearrange("p a b -> p (a b)"), in_=trpB, func=AF.Identity), r=["ps0"], w=["tT"])
                    for nb in range(2):
                        for kc in range(8):
                            P.add("pe", I('matmul', out=pb(2 + nb), lhsT=tT[:, kc, :], rhs=wpb[:, kc, 512 * nb:512 * (nb + 1)],
                                                                        start=(kc == 0), stop=(kc == 7)), r=["tT", "wpb"], w=["ps%d" % (2 + nb)])
                    P.add("dve", I('tensor_tensor', out=t1d[:], in0=pb(2, 2), in1=sgb[:], op=ALU.mult), r=["ps2", "ps3", "sgb"], w=["t1"])
                    P.add("dve", I('tensor_tensor', out=mg[:], in0=t1d[:], in1=apt[:], op=ALU.add), r=["t1", "apt"], w=["mg"])
                    if CUT <= 5:
                        return
                    for kc in range(8):
                        P.add("pe", I('transpose', out=trpC[:, 128 * kc:128 * (kc + 1)], in_=mg[:, 128 * kc:128 * (kc + 1)], identity=ident_bf[:]),
                              r=["mg", "identbf"], w=["ps1"])
                    P.add("act", I('activation', out=tT[:].rearrange("p a b -> p (a b)"), in_=trpC, func=AF.Identity), r=["ps1"], w=["tT"])
                    for nb in range(2):
                        for kc in range(8):
                            P.add("pe", I('matmul', out=pb(nb), lhsT=tT[:, kc, :], rhs=wout[:, kc, 512 * nb:512 * (nb + 1)],
                                                                        start=(kc == 0), stop=(kc == 7)), r=["tT", "wout"], w=["ps%d" % nb])
                    P.add("dve", I('tensor_tensor', out=t1d[:], in0=pb(0, 2), in1=gbc[:], op=ALU.mult), r=["ps0", "ps1", gkey], w=["t1"])
                    P.add("dve", I('tensor_tensor', out=t1d[:], in0=t1d[:], in1=xt[:], op=ALU.add), r=["t1", "xt"], w=["t1"])
                    P.add("pool", I('dma_start', out=xs_tile(t), in_=t1d[:]), r=["t1"], w=["xs%d" % t], dma=True)

                if has_ctx:
                    P.add("dve", I('memset', Sf[:].rearrange("p a b -> p (a b)"), 0.0), w=["Sf"])
                    fwd_tile(NT, 1, NT, True, False)
                    fwd_tile(NT + 1, 1, NT, False, True)
                P.add("dve", I('tensor_copy', out=Sf[:].rearrange("p a b -> p (a b)"), in_=scf[:].rearrange("p a b -> p (a b)")),
                      r=["scf"], w=["Sf"])
                P.add("act", I('activation', out=Sfb[:].rearrange("p a b -> p (a b)"), in_=Sf[:].rearrange("p a b -> p (a b)"),
                                                    func=AF.Identity), r=["Sf"], w=["Sfb"])
                P.local = {"xt", "hT", "apt", "Tbb", "rt", "t1", "t2", "qkr", "qT", "qTf", "qTb", "kT", "kd", "vb", "sgr", "sgb",
                           "PT", "yb", "tT", "mg", "lnst", "lnmv"}
                P.barrier()
                two_stream(list(range(NT)), lambda c: fwd_tile(c, 0, c, False, False))

            if stop_after == ('B', layer):
                raise _Stop
            P.barrier()
            with Scope() as ph:
                slots_i = sb("slots_i", [128, 68], I32, ph)
                wts = sb("wts", [128, 34, 2], F32, ph)
                counts_i = sb("counts_i", [128, 32], I32, ph)
                gt2_bc = [sb("gt2bc%d" % i, [128, D], F32, ph) for i in range(2 if has_ctx else 1)]
                with Scope() as ph1:
                    A2bc = [sb("A2bc%d" % i, [128, D], F32, ph1) for i in range(2 if has_ctx else 1)]
                    B2bc = [sb("B2bc%d" % i, [128, D], F32, ph1) for i in range(2 if has_ctx else 1)]
                    wr = sb("wr", [128, 8, 36], F32, ph1)
                    br_bc = sb("br_bc", [128, 36], F32, ph1)
                    xt = sb2("xt", [128, D], F32, ph1)
                    junk = sb2("junk", [128, D], BF16, ph1)
                    h2f = sb2("h2f", [128, D], F32, ph1)
                    h2b = sb2("h2b", [128, D], BF16, ph1)
                    h2T = sb2("h2T", [128, 8, 128], F32, ph1)
                    lgt = sb2("lgt", [128, 36], F32, ph1)
                    msk = sb2("msk", [128, 32], F32, ph1)
                    sel = sb2("sel", [128, 32], F32, ph1)
                    oh1 = sb2("oh1", [128, 32], F32, ph1)
                    oh2 = sb2("oh2", [128, 32], F32, ph1)
                    posm = sb2("posm", [128, 32], F32, ph1)
                    ovr = sb2("ovr", [128, 32], F32, ph1)
                    run_bc = sb("run_bc", [128, 32], F32, ph1)
                    mx8 = sb2("mx8", [128, 8], F32, ph1)
                    sm = sb2("sm", [128, 16], F32, ph1)
                    for i in range(len(gt2_bc)):
                        bcast_gate(gt2_bc[i][:], "gt2bc%d" % i, 5, i)
                        bcast_feat(A2bc[i][:], "A2bc%d" % i, a2, "a2", i)
                        bcast_gate(B2bc[i][:], "B2bc%d" % i, 3, i)
                    for kc in range(8):
                        P.add("sp", I('dma_start', out=wr[:, kc, :], in_=w_r[L, 128 * kc:128 * (kc + 1), :]), w=["wr"], dma=True)
                    P.add("sp", I('dma_start', out=br_bc[:], in_=b_r[L].partition_broadcast(128)), w=["br_bc"], dma=True)
                    P.add("dve", I('memset', run_bc[:], 0.0), w=["run_bc"])
                    def tileM1(t):
                        cond = 0 if t < NT else 1
                        P.add("sp", I('dma_start', out=xt[:], in_=xs_tile(t)), r=["xs%d" % t], w=["xt"], dma=True)
                        c0, k = rms_rstd(xt[:], "xt", junk[:], "junk", 2 + CUR["st"])
                        P.add("dve", I('scalar_tensor_tensor', out=h2f[:], in0=xt[:], scalar=c0, in1=A2bc[cond][:],
                                                                                 op0=ALU.mult, op1=ALU.mult), r=["xt", k, "A2bc%d" % cond], w=["h2f"])
                        P.add("dve", I('tensor_tensor', out=h2f[:], in0=h2f[:], in1=B2bc[cond][:], op=ALU.add),
                              r=["h2f", "B2bc%d" % cond], w=["h2f"])
                        P.add("act", I('activation', out=h2b[:], in_=h2f[:], func=AF.Identity), r=["h2f"], w=["h2b"])
                        for kc in range(8):
                            P.add("pe", I('transpose', out=pb(kc // 4)[:, 128 * (kc % 4):128 * (kc % 4 + 1)],
                                                                     in_=h2f[:, 128 * kc:128 * (kc + 1)], identity=ident),
                                  r=["h2f", "cst"], w=["ps%d" % (kc // 4)])
                        P.add("act", I('activation', out=h2T[:].rearrange("p a b -> p (a b)"), in_=pb(0, 2), func=AF.Identity),
                              r=["ps0", "ps1"], w=["h2T"])
                        for kc in range(8):
                            P.add("pe", I('matmul', out=pb(2)[:, 0:36], lhsT=h2T[:, kc, :], rhs=wr[:, kc, :],
                                                                  start=(kc == 0), stop=(kc == 7)), r=["h2T", "wr"], w=["ps2"])
                        P.add("dve", I('tensor_tensor', out=lgt[:], in0=pb(2)[:, 0:36], in1=br_bc[:], op=ALU.add),
                              r=["ps2", "br_bc"], w=["lgt"])
                        gmax = sm[:, 0:1]
                        P.add("dve", I('reduce_max', out=gmax, in_=lgt[:, 0:4], axis=AX.X), r=["lgt"], w=["sm0"])
                        P.add("dve", I('tensor_scalar', out=sm[:, 4:8], in0=lgt[:, 0:4], scalar1=gmax, scalar2=None, op0=ALU.is_ge),
                              r=["lgt", "sm0"], w=["goh"])
                        P.add("dve", I('tensor_scalar', out=sm[:, 1:2], in0=gmax, scalar1=-1.0, scalar2=None, op0=ALU.mult),
                              r=["sm0"], w=["sm1"])
                        P.add("act", I('activation', out=sm[:, 8:12], in_=lgt[:, 0:4], func=AF.Exp, bias=sm[:, 1:2], accum_out=sm[:, 2:3]),
                              r=["lgt", "sm1"], w=["sm2", "gexp"])
                        P.add("dve", I('reciprocal', out=sm[:, 2:3], in_=sm[:, 2:3]), r=["sm2"], w=["sm2"])
                        P.add("dve", I('tensor_scalar', out=sm[:, 4:8], in0=sm[:, 4:8], scalar1=1e30, scalar2=-1e30, op0=ALU.mult, op1=ALU.add),
                              r=["goh"], w=["goh"])
                        for g in range(4):
                            P.add("dve", I('tensor_scalar', out=msk[:, 8 * g:8 * (g + 1)], in0=lgt[:, 4 + 8 * g:12 + 8 * g],
                                                                        scalar1=sm[:, 4 + g:5 + g], scalar2=None, op0=ALU.add),
                                  r=["lgt", "goh"], w=["msk"])
                        P.add("dve", I('max', out=mx8[:], in_=msk[:]), r=["msk"], w=["mx8"])
                        P.add("dve", I('tensor_scalar', out=oh1[:], in0=msk[:], scalar1=mx8[:, 0:1], scalar2=None, op0=ALU.is_ge),
                              r=["msk", "mx8"], w=["oh1"])
                        P.add("dve", I('tensor_scalar', out=sel[:], in0=msk[:], scalar1=mx8[:, 1:2], scalar2=None, op0=ALU.is_ge),
                              r=["msk", "mx8"], w=["sel"])
                        P.add("dve", I('tensor_tensor', out=oh2[:], in0=sel[:], in1=oh1[:], op=ALU.subtract), r=["sel", "oh1"], w=["oh2"])
                        P.add("dve", I('tensor_tensor', out=sm[:, 3:4], in0=mx8[:, 1:2], in1=mx8[:, 0:1], op=ALU.subtract), r=["mx8"], w=["sm3"])
                        P.add("act", I('activation', out=sm[:, 3:4], in_=sm[:, 3:4], func=AF.Exp), r=["sm3"], w=["sm3"])
                        P.add("dve", I('tensor_scalar', out=sm[:, 12:13], in0=sm[:, 3:4], scalar1=1.0, scalar2=None, op0=ALU.add), r=["sm3"], w=["sm12"])
                        P.add("dve", I('reciprocal', out=sm[:, 12:13], in_=sm[:, 12:13]), r=["sm12"], w=["sm12"])
                        P.add("dve", I('tensor_tensor', out=wts[:, t, 0:1], in0=sm[:, 12:13], in1=sm[:, 2:3], op=ALU.mult),
                              r=["sm12", "sm2"], w=["wts"])
                        P.add("dve", I('tensor_tensor', out=wts[:, t, 1:2], in0=wts[:, t, 0:1], in1=sm[:, 3:4], op=ALU.mult),
                              r=["wts", "sm3"], w=["wts"])
                        P.add("pe", I('matmul', out=pb(3)[:, 0:32], lhsT=ltri, rhs=sel[:], start=True, stop=True), r=["sel", "cst"], w=["ps3"])
                        P.add("pe", I('matmul', out=pb(3)[:, 32:64], lhsT=ones, rhs=sel[:], start=True, stop=True), r=["sel", "cst"], w=["ps3"])
                        P.add("dve", I('tensor_tensor', out=posm[:], in0=pb(3)[:, 0:32], in1=run_bc[:], op=ALU.add), r=["ps3", "run_bc"], w=["posm"])
                        P.add("dve", I('tensor_tensor', out=run_bc[:], in0=pb(3)[:, 32:64], in1=run_bc[:], op=ALU.add), r=["ps3", "run_bc", "posm"], w=["run_bc"])
                        P.add("dve", I('tensor_scalar', out=ovr[:], in0=posm[:], scalar1=float(CAP), scalar2=BIG, op0=ALU.is_ge, op1=ALU.mult),
                              r=["posm"], w=["ovr"])
                        P.add("dve", I('tensor_tensor', out=posm[:], in0=posm[:], in1=cst[:, C_ECAP:C_ECAP + 32], op=ALU.add), r=["posm", "cst"], w=["posm"])
                        P.add("dve", I('tensor_tensor', out=posm[:], in0=posm[:], in1=ovr[:], op=ALU.add), r=["posm", "ovr"], w=["posm"])
                        P.add("dve", I('tensor_tensor', out=oh1[:], in0=oh1[:], in1=posm[:], op=ALU.mult), r=["oh1", "posm", "oh2"], w=["oh1"])
                        P.add("dve", I('tensor_tensor', out=oh2[:], in0=oh2[:], in1=posm[:], op=ALU.mult), r=["oh2", "posm"], w=["oh2"])
                        P.add("dve", I('reduce_sum', out=sm[:, 13:14], in_=oh1[:], axis=AX.X), r=["oh1"], w=["sm13"])
                        P.add("dve", I('reduce_sum', out=sm[:, 14:15], in_=oh2[:], axis=AX.X), r=["oh2"], w=["sm13"])
                        P.add("dve", I('tensor_copy', out=slots_i[:, 2 * t:2 * t + 2], in_=sm[:, 13:15]), r=["sm13"], w=["slots"])
                        for kk in range(2):
                            P.add("pool", I('indirect_dma_start',
                                out=Xs[:, :], out_offset=bass.IndirectOffsetOnAxis(ap=slots_i[:, 2 * t + kk:2 * t + kk + 1], axis=0),
                                in_=h2b[:, :], in_offset=None, bounds_check='BCREG', oob_is_err=False),
                                r=["h2b", "slots"], w=["Xs_%d_%d" % (t, kk)], dma=True)

                    P.local = {"xt", "junk", "h2f", "h2b", "h2T", "lgt", "msk", "sel", "oh1", "oh2", "posm", "ovr", "mx8",
                               "sm0", "sm1", "sm2", "sm3", "goh", "gexp", "sm12", "sm13", "rstat2", "rstat3"}
                    P.barrier()
                    two_stream(tiles_all, tileM1)

                P.add("dve", I('tensor_copy', out=counts_i[:], in_=run_bc[:]), r=["run_bc"], w=["counts_i"])
                P.barrier()
                if debug:
                    P.add("sp", I('dma_start', out=dbgt[:, 0:68], in_=wts[:].rearrange("p a b -> p (a b)")), r=["wts"], w=["dbg0"], dma=True)
                    P.add("sp", I('dma_start', out=dbgt[:, 68:136], in_=slots_i[:].bitcast(F32)), r=["slots"], w=["dbg1"], dma=True)
                    P.add("sp", I('dma_start', out=dbgt[:, 136:1160], in_=gt2_bc[0][:]), r=["gt2bc0"], w=["dbg2"], dma=True)
                with Scope() as ph2:
                    w1b = [sb("w1b%d" % i, [128, 8, FF], BF16, ph2) for i in range(2)]
                    w3b = [sb("w3b%d" % i, [128, 8, FF], BF16, ph2) for i in range(2)]
                    w2b = [sb("w2b%d" % i, [128, 4, D], BF16, ph2) for i in range(2)]
                    xg = [sb("xg%d" % i, [128, GS // 128, D], BF16, ph2) for i in range(2)]
                    XTs = [sb("XT%d" % i, [128, 8, GS], BF16, ph2) for i in range(2)]
                    sils = [sb("sil%d" % i, [128, GS], F32, ph2) for i in range(2)]
                    actTs = [sb("actT%d" % i, [128, 4, GS], BF16, ph2) for i in range(2)]
                    ysb = [sb("ysb%d" % i, [128, D], F32, ph2) for i in range(2)]
                    it = 0
                    for ex in range(NE):
                        wb_ = ex % 2
                        for exl in ([0, 1] if ex == 0 else ([ex + 1] if ex + 1 < NE else [])):
                            wl_ = exl % 2
                            P.add("pool", I('dma_start', out=w1b[wl_][:, :, :], in_=w1[L, exl].rearrange("(k p) f -> p k f", p=128)),
                                  w=["w1b%d" % wl_], dma=True)
                            P.add("pool", I('dma_start', out=w3b[wl_][:, :, :], in_=w3[L, exl].rearrange("(k p) f -> p k f", p=128)),
                                  w=["w3b%d" % wl_], dma=True)
                            P.add("pool", I('dma_start', out=w2b[wl_][:, :, :], in_=w2[L, exl].rearrange("(k p) f -> p k f", p=128)),
                                  w=["w2b%d" % wl_], dma=True)
                        for grp in range(CAP // GS):
                            P.begin_region(counts_i[0:1, ex:ex + 1], grp * GS, "counts_i")
                            xb_ = it % 2
                            XT = XTs[xb_]
                            actT = actTs[xb_]
                            XK = "XT%d" % xb_
                            AK = "actT%d" % xb_
                            it += 1
                            base = ex * CAP + grp * GS
                            for j in range(GS // 128):
                                P.add("sp", I('dma_start', out=xg[xb_][:, j, :], in_=Xs[base + 128 * j:base + 128 * (j + 1), :]),
                                      w=["xg%d_%d" % (xb_, j)], dma=True)
                            for j in range(GS // 128):
                                bank = j % 2
                                trp = pb(bank).bitcast(BF16)
                                for kc in range(8):
                                    P.add("pe", I('transpose', out=trp[:, 128 * kc:128 * (kc + 1)],
                                                                                                    in_=xg[xb_][:, j, 128 * kc:128 * (kc + 1)], identity=ident_bf[:]),
                                          r=["xg%d_%d" % (xb_, j), "identbf"], w=["ps%d" % bank])
                                P.add("dve", I('tensor_copy', out=XT[:, :, 128 * j:128 * (j + 1)], in_=trp.rearrange("p (a b) -> p a b", a=8)),
                                      r=["ps%d" % bank], w=[XK])
                            for fc in range(4):
                                b1 = 2 + 2 * (fc % 2)
                                for kc in range(8):
                                    P.add("pe", I('matmul', out=pb(b1)[:, 0:GS], lhsT=w1b[wb_][:, kc, 128 * fc:128 * (fc + 1)], rhs=XT[:, kc, :],
                                                                                                start=(kc == 0), stop=(kc == 7)), r=[XK, "w1b%d" % wb_], w=["ps%d" % b1])
                                for kc in range(8):
                                    P.add("pe", I('matmul', out=pb(b1 + 1)[:, 0:GS], lhsT=w3b[wb_][:, kc, 128 * fc:128 * (fc + 1)], rhs=XT[:, kc, :],
                                                                                                start=(kc == 0), stop=(kc == 7)), r=[XK, "w3b%d" % wb_], w=["ps%d" % (b1 + 1)])
                                sil = sils[fc % 2]
                                SK = "sil%d" % (fc % 2)
                                P.add("act", I('activation', out=sil[:], in_=pb(b1)[:, 0:GS], func=AF.Silu), r=["ps%d" % b1], w=[SK])
                                P.add("dve", I('tensor_tensor', out=actT[:, fc, :], in0=pb(b1 + 1)[:, 0:GS], in1=sil[:], op=ALU.mult),
                                      r=["ps%d" % (b1 + 1), SK], w=[AK])
                            for j in range(GS // 128):
                                yb_ = j % 2
                                for nb in range(2):
                                    for fc in range(4):
                                        P.add("pe", I('matmul', out=pb(6 + nb), lhsT=actT[:, fc, 128 * j:128 * (j + 1)],
                                                                                                  rhs=w2b[wb_][:, fc, 512 * nb:512 * (nb + 1)], start=(fc == 0), stop=(fc == 3)),
                                              r=[AK, "w2b%d" % wb_], w=["ps%d" % (6 + nb)])
                                P.add("dve", I('tensor_copy', out=ysb[yb_][:], in_=pb(6, 2)), r=["ps6", "ps7"], w=["ysb%d" % yb_])
                                P.add("sp", I('dma_start', out=Yall[base + 128 * j:base + 128 * (j + 1), :], in_=ysb[yb_][:]),
                                      r=["ysb%d" % yb_], w=["Yall_%d_%d" % (base, j)], dma=True)
                            P.end_region()

                P.barrier()
                with Scope() as ph3:
                    xt = sb2("xt", [128, D], F32, ph3)
                    y0 = sb2("y0", [128, D], F32, ph3)
                    y1 = sb2("y1", [128, D], F32, ph3)
                    junk = sb2("junk", [128, D], BF16, ph3)
                    last = (layer == n_layers - 1)
                    if last:
                        gfin_bc = sb("gfin_bc", [128, D], F32, ph3)
                        P.add("sp", I('dma_start', out=gfin_bc[:], in_=g_fin.partition_broadcast(128)), w=["gfin"], dma=True)
                    def tileM3(t):
                        cond = 0 if t < NT else 1
                        P.add("sp", I('dma_start', out=xt[:], in_=xs_tile(t)), r=["xs%d" % t], w=["xt"], dma=True)
                        P.add("dve", I('memset', y0[:], 0.0), w=["y0"])
                        P.add("dve", I('memset', y1[:], 0.0), w=["y1"])
                        for kk, yy, yk in ((0, y0, "y0"), (1, y1, "y1")):
                            P.add("pool", I('indirect_dma_start',
                                out=yy[:, :], out_offset=None, in_=Yall[:, :],
                                in_offset=bass.IndirectOffsetOnAxis(ap=slots_i[:, 2 * t + kk:2 * t + kk + 1], axis=0),
                                bounds_check='BCREG', oob_is_err=False), r=["slots"], w=[yk], dma=True)
                        if debug and t == 0:
                            P.add("sp", I('dma_start', out=dbgt[:, 1160:2184], in_=y0[:]), r=["y0"], w=["dbg3"], dma=True)
                        P.add("dve", I('tensor_scalar', out=y0[:], in0=y0[:], scalar1=wts[:, t, 0:1], scalar2=None, op0=ALU.mult),
                              r=["y0", "wts"], w=["y0"])
                        P.add("dve", I('scalar_tensor_tensor', out=y0[:], in0=y1[:], scalar=wts[:, t, 1:2], in1=y0[:], op0=ALU.mult, op1=ALU.add),
                              r=["y0", "y1", "wts"], w=["y0"])
                        P.add("dve", I('tensor_tensor', out=y0[:], in0=y0[:], in1=gt2_bc[cond][:], op=ALU.mult),
                              r=["y0", "gt2bc%d" % cond], w=["y0"])
                        P.add("dve", I('tensor_tensor', out=y0[:], in0=y0[:], in1=xt[:], op=ALU.add), r=["y0", "xt"], w=["y0"])
                        if not last:
                            P.add("pool", I('dma_start', out=xs_tile(t), in_=y0[:]), r=["y0"], w=["xs%d" % t], dma=True)
                        elif t < NT:
                            c0, k = rms_rstd(y0[:], "y0", junk[:], "junk", 2 + CUR["st"])
                            P.add("dve", I('scalar_tensor_tensor', out=y1[:], in0=y0[:], scalar=c0, in1=gfin_bc[:], op0=ALU.mult, op1=ALU.mult),
                                  r=["y0", k, "gfin"], w=["y1"])
                            P.add("pool", I('dma_start', out=out_d[128 * t:128 * (t + 1), :], in_=y1[:]), r=["y1"], w=["out%d" % t], dma=True)

                    P.local = {"xt", "y0", "y1", "junk", "rstat2", "rstat3"}
                    extra = None
                    if layer + 1 < n_layers:
                        wst1 = [sb("wadan%d" % i, [128, 8, 512], F32, ph3) for i in range(2)]
                        P.rec = []
                        phase0(layer + 1, wst1)
                        extra = P.rec
                        P.rec = None
                    two_stream(tiles_all, tileM3, extra=extra)

          except _Stop:
            break

        P.stats_peak = peak[0]
        with nc.Block() as block:
            P.emit(nc, block, es)
    return nc, P


_CACHE = {}


def prep_inputs(inputs):
    f = lambda a: np.ascontiguousarray(a, dtype=np.float32)
    c = inputs["c"]
    c_ctx = inputs["c_ctx"]
    shared = {
        "w_ada": f(inputs["w_ada"]),
        "b_adaT": f(inputs["b_ada"].reshape(2, 48, 128).transpose(0, 2, 1)),
        "g_mixT": f(inputs["g_mix"].reshape(2, 8, 128).transpose(0, 2, 1)),
        "g_ffnT": f(inputs["g_ffn"].reshape(2, 8, 128).transpose(0, 2, 1)),
        "g_sguT": f(inputs["g_sgu"].reshape(2, 8, 128).transpose(0, 2, 1)),
        "g_final": f(inputs["g_final"].reshape(1, D)),
        "w_in": f(inputs["w_in"]),
        "w_sT": f(inputs["w_s"].transpose(0, 1, 3, 2)),
        "b_s": f(inputs["b_s"].reshape(2, 1, D)),
        "decay_logit": f(inputs["decay_logit"].reshape(2, 1, 8)),
        "w_pa": f(inputs["w_pa"]),
        "w_pb": f(inputs["w_pb"]),
        "w_out": f(inputs["w_out"]),
        "w_r": f(np.concatenate([inputs["w_group"], inputs["w_erouter"]], axis=2)),
        "b_r": f(np.concatenate([inputs["b_group"], inputs["b_erouter"]], axis=1).reshape(2, 1, 36)),
        "w1": f(inputs["w1"]),
        "w3": f(inputs["w3"]),
        "w2": f(inputs["w2"]),
        "consts": make_consts(),
        "rot": make_rot(),
    }
    in_maps = []
    for b in range(8):
        m = dict(shared)
        m["x"] = f(inputs["x"][b])
        m["ctx"] = f(inputs["ctx"][b])
        cv = np.stack([np.asarray(c[b]).reshape(8, 128).T, np.asarray(c_ctx).reshape(8, 128).T], axis=-1)
        m["cvec"] = f(cv)
        in_maps.append(m)
    return in_maps


def kernel(**inputs):
    inputs = {k: np.asarray(v) for k, v in inputs.items()}
    if "nc" not in _CACHE:
        _CACHE["nc"] = build()[0]
    nc = _CACHE["nc"]
    in_maps = prep_inputs(inputs)
    res = run_bass_kernel_spmd(nc, in_maps, core_ids=list(range(8)))
    out = np.stack([np.asarray(r["out"], dtype=np.float32) for r in res.results], axis=0)
    return out
```
